# Optimizing a Trainium2 kernel written in Bass

```python
import jax, jax.numpy as jnp
from jax import lax
import numpy as np

D_MODEL = 1024
BATCH = 4
SEQ = 8192
DEPTH = 4

N_MEM = 256
D_FF = 2816
EPS = 1e-6
GLA_HEADS = 4
GLA_DK = D_MODEL // 2 // GLA_HEADS
GLA_DV = D_MODEL // GLA_HEADS
GLA_GATE_RANK = 16
GLA_GATE_TAU = 16.0
GLA_CHUNK = 64
MLA_HEADS = 8
MLA_Q_RANK = 384
MLA_KV_RANK = 256
MLA_NOPE = 128
MLA_ROPE = 64
MLA_DV = 128
ROPE_BASE = 10000.0
Q_BLOCK = 128
MEM_HEADS = 4
MEM_DH = 128
MEM_Q_WIDTH = MEM_HEADS * MEM_DH
MIX_WIDTH = D_MODEL + MEM_Q_WIDTH
GLA_WIDTHS = (GLA_HEADS * GLA_DK, GLA_HEADS * GLA_DK, GLA_HEADS * GLA_DV,
              GLA_GATE_RANK, GLA_HEADS * GLA_DV, MEM_Q_WIDTH)
GLA_IN = sum(GLA_WIDTHS)
GLA_SPLITS = tuple(int(c) for c in np.cumsum(GLA_WIDTHS)[:-1])
MLA_WIDTHS = (MLA_Q_RANK, MLA_KV_RANK, MLA_ROPE, MEM_Q_WIDTH)
MLA_IN = sum(MLA_WIDTHS)
MLA_SPLITS = tuple(int(c) for c in np.cumsum(MLA_WIDTHS)[:-1])
N_GLA = (DEPTH + 1) // 2
N_MLA = DEPTH // 2

kernel_name = "hybrid_gla_mla_macaron_memory_trunk"


def rmsnorm(x, g):
    xf = x.astype(jnp.float32)
    y = xf * lax.rsqrt(jnp.mean(xf * xf, axis=-1, keepdims=True) + EPS)
    return (y * g.astype(jnp.float32)).astype(x.dtype)


def swiglu(h, w_gate, w_up, w_down):
    return (jax.nn.silu(h @ w_gate) * (h @ w_up)) @ w_down


def rope_tables(seq, dim):
    inv = 1.0 / (ROPE_BASE ** (jnp.arange(0, dim, 2, dtype=jnp.float32) / dim))
    ang = jnp.arange(seq, dtype=jnp.float32)[:, None] * inv[None, :]
    return jnp.cos(ang)[:, None, :], jnp.sin(ang)[:, None, :]


def apply_rope(t, cos, sin):
    t1, t2 = jnp.split(t, 2, axis=-1)
    c = cos.astype(t.dtype)
    s = sin.astype(t.dtype)
    return jnp.concatenate([t1 * c - t2 * s, t1 * s + t2 * c], axis=-1)


def gla_attention(q, k, v, log_a):
    B, S, H, DK = q.shape
    DV = v.shape[-1]
    C = GLA_CHUNK
    N = S // C
    f32 = jnp.float32

    def to_chunks(t):
        return t.reshape(B, N, C, H, t.shape[-1]).transpose(0, 1, 3, 2, 4)

    qc = to_chunks(q.astype(f32)) * (DK ** -0.5)
    kc = to_chunks(k.astype(f32))
    vc = to_chunks(v.astype(f32))
    bc = jnp.cumsum(to_chunks(log_a.astype(f32)), axis=3)
    b_last = bc[:, :, :, C - 1:C, :]
    b_ref = bc[:, :, :, C // 2 - 1:C // 2, :]

    scores = jnp.einsum('bnhid,bnhjd->bnhij', qc * jnp.exp(bc - b_ref), kc * jnp.exp(b_ref - bc))
    causal = jnp.tril(jnp.ones((C, C), dtype=bool))
    scores = jnp.where(causal, scores, 0.0)
    o_intra = jnp.einsum('bnhij,bnhjv->bnhiv', scores, vc)

    u = jnp.einsum('bnhjd,bnhjv->bnhdv', kc * jnp.exp(b_last - bc), vc)
    decay = jnp.exp(b_last[:, :, :, 0, :])

    def step(s, inp):
        d_n, u_n = inp
        return d_n[..., None] * s + u_n, s

    s0 = jnp.zeros((B, H, DK, DV), f32)
    _, s_prev = lax.scan(step, s0, (decay.transpose(1, 0, 2, 3), u.transpose(1, 0, 2, 3, 4)))
    s_prev = s_prev.transpose(1, 0, 2, 3, 4)
    o_inter = jnp.einsum('bnhid,bnhdv->bnhiv', qc * jnp.exp(bc), s_prev)
    o = (o_intra + o_inter).transpose(0, 1, 3, 2, 4).reshape(B, S, H, DV)
    return o.astype(v.dtype)


def causal_block_attention(q, k, v, scale):
    B, H, S, _ = q.shape
    nb = S // Q_BLOCK
    qb = q.reshape(B, H, nb, Q_BLOCK, q.shape[-1]).transpose(2, 0, 1, 3, 4)
    kpos = jnp.arange(S)

    def one_block(args):
        i, q_i = args
        s = jnp.einsum('bhqd,bhkd->bhqk', q_i, k).astype(jnp.float32) * scale
        qpos = i * Q_BLOCK + jnp.arange(Q_BLOCK)
        s = jnp.where(kpos[None, :] <= qpos[:, None], s, -jnp.inf)
        p = jax.nn.softmax(s, axis=-1).astype(v.dtype)
        return jnp.einsum('bhqk,bhkd->bhqd', p, v)

    o = lax.map(one_block, (jnp.arange(nb), qb))
    return o.transpose(1, 2, 0, 3, 4).reshape(B, H, S, v.shape[-1])


def mla_attention(c_q, c_kv, k_r, q_norm, kv_norm, w_uq, w_ukv,
                  qn_norm, qr_norm, kn_norm, kr_norm, cos, sin):
    B, S, _ = c_q.shape
    q = (rmsnorm(c_q, q_norm) @ w_uq).reshape(B, S, MLA_HEADS, MLA_NOPE + MLA_ROPE)
    kv = (rmsnorm(c_kv, kv_norm) @ w_ukv).reshape(B, S, MLA_HEADS, MLA_NOPE + MLA_DV)
    q_nope = rmsnorm(q[..., :MLA_NOPE], qn_norm)
    q_rope = apply_rope(rmsnorm(q[..., MLA_NOPE:], qr_norm), cos, sin)
    k_nope = rmsnorm(kv[..., :MLA_NOPE], kn_norm)
    v = kv[..., MLA_NOPE:]
    k_rope = apply_rope(rmsnorm(k_r, kr_norm)[:, :, None, :], cos, sin)
    k_rope = jnp.broadcast_to(k_rope, (B, S, MLA_HEADS, MLA_ROPE))
    qh = jnp.concatenate([q_nope, q_rope], axis=-1).transpose(0, 2, 1, 3)
    kh = jnp.concatenate([k_nope, k_rope], axis=-1).transpose(0, 2, 1, 3)
    vh = v.transpose(0, 2, 1, 3)
    o = causal_block_attention(qh, kh, vh, (MLA_NOPE + MLA_ROPE) ** -0.5)
    return o.transpose(0, 2, 1, 3).reshape(B, S, MLA_HEADS * MLA_DV)


def memory_cross_attention(q_mem, mem_h, w_kv, q_gain, k_gain):
    B, S, _ = q_mem.shape
    M = mem_h.shape[1]
    k, v = jnp.split(mem_h @ w_kv, 2, axis=-1)
    q = rmsnorm(q_mem.reshape(B, S, MEM_HEADS, MEM_DH), q_gain)
    k = rmsnorm(k.reshape(B, M, MEM_HEADS, MEM_DH), k_gain)
    v = v.reshape(B, M, MEM_HEADS, MEM_DH)
    s = jnp.einsum('bshd,bmhd->bhsm', q, k).astype(jnp.float32) * (MEM_DH ** -0.5)
    p = jax.nn.softmax(s, axis=-1).astype(v.dtype)
    return jnp.einsum('bhsm,bmhd->bshd', p, v).reshape(B, S, MEM_HEADS * MEM_DH)


def setup_inputs(seed: int = 0) -> dict:
    key = jax.random.key(seed)
    ks = iter(jax.random.split(key, 48))

    def w(shape, fan_in):
        return jax.random.normal(next(ks), shape, jnp.float32) * (fan_in ** -0.5)

    def gain(shape):
        return 1.0 + 0.02 * jax.random.normal(next(ks), shape, jnp.float32)

    def bias(shape, scale):
        return scale * jax.random.normal(next(ks), shape, jnp.float32)

    return {
        "x": jax.random.normal(next(ks), (BATCH, SEQ, D_MODEL), jnp.float32),
        "mem": jax.random.normal(next(ks), (BATCH, N_MEM, D_MODEL), jnp.float32),
        "ffn1_norm": gain((DEPTH, D_MODEL)),
        "ffn1_w_gate": w((DEPTH, D_MODEL, D_FF), D_MODEL),
        "ffn1_w_up": w((DEPTH, D_MODEL, D_FF), D_MODEL),
        "ffn1_w_down": w((DEPTH, D_FF, D_MODEL), D_FF),
        "ffn2_norm": gain((DEPTH, D_MODEL)),
        "ffn2_w_gate": w((DEPTH, D_MODEL, D_FF), D_MODEL),
        "ffn2_w_up": w((DEPTH, D_MODEL, D_FF), D_MODEL),
        "ffn2_w_down": w((DEPTH, D_FF, D_MODEL), D_FF),
        "mix_norm": gain((DEPTH, D_MODEL)),
        "w_out": w((DEPTH, MIX_WIDTH, D_MODEL), MIX_WIDTH),
        "mem_norm": gain((DEPTH, D_MODEL)),
        "mem_w_kv": w((DEPTH, D_MODEL, 2 * MEM_Q_WIDTH), D_MODEL),
        "memq_norm": gain((DEPTH, MEM_DH)),
        "memk_norm": gain((DEPTH, MEM_DH)),
        "gla_w_in": w((N_GLA, D_MODEL, GLA_IN), D_MODEL),
        "gla_w_alpha": w((N_GLA, GLA_GATE_RANK, GLA_HEADS * GLA_DK), GLA_GATE_RANK),
        "gla_b_alpha": bias((N_GLA, GLA_HEADS * GLA_DK), 0.1),
        "gla_out_norm": gain((N_GLA, GLA_DV)),
        "mla_w_in": w((N_MLA, D_MODEL, MLA_IN), D_MODEL),
        "mla_q_norm": gain((N_MLA, MLA_Q_RANK)),
        "mla_kv_norm": gain((N_MLA, MLA_KV_RANK)),
        "mla_w_uq": w((N_MLA, MLA_Q_RANK, MLA_HEADS * (MLA_NOPE + MLA_ROPE)), MLA_Q_RANK),
        "mla_w_ukv": w((N_MLA, MLA_KV_RANK, MLA_HEADS * (MLA_NOPE + MLA_DV)), MLA_KV_RANK),
        "mla_qn_norm": gain((N_MLA, MLA_NOPE)),
        "mla_qr_norm": gain((N_MLA, MLA_ROPE)),
        "mla_kn_norm": gain((N_MLA, MLA_NOPE)),
        "mla_kr_norm": gain((N_MLA, MLA_ROPE)),
    }


def reference(x, mem, ffn1_norm, ffn1_w_gate, ffn1_w_up, ffn1_w_down,
              ffn2_norm, ffn2_w_gate, ffn2_w_up, ffn2_w_down,
              mix_norm, w_out, mem_norm, mem_w_kv, memq_norm, memk_norm,
              gla_w_in, gla_w_alpha, gla_b_alpha, gla_out_norm,
              mla_w_in, mla_q_norm, mla_kv_norm, mla_w_uq, mla_w_ukv,
              mla_qn_norm, mla_qr_norm, mla_kn_norm, mla_kr_norm):
    B, S, _ = x.shape
    cos, sin = rope_tables(S, MLA_ROPE)
    for i in range(DEPTH):
        x = x + 0.5 * swiglu(rmsnorm(x, ffn1_norm[i]), ffn1_w_gate[i], ffn1_w_up[i], ffn1_w_down[i])
        h = rmsnorm(x, mix_norm[i])
        j = i // 2
        if i % 2 == 0:
            q, k, v, a_low, r, q_mem = jnp.split(h @ gla_w_in[j], GLA_SPLITS, axis=-1)
            log_a = jax.nn.log_sigmoid(a_low @ gla_w_alpha[j] + gla_b_alpha[j]) / GLA_GATE_TAU
            o = gla_attention(q.reshape(B, S, GLA_HEADS, GLA_DK),
                              k.reshape(B, S, GLA_HEADS, GLA_DK),
                              v.reshape(B, S, GLA_HEADS, GLA_DV),
                              log_a.reshape(B, S, GLA_HEADS, GLA_DK))
            o = rmsnorm(o, gla_out_norm[j]).reshape(B, S, GLA_HEADS * GLA_DV) * jax.nn.silu(r)
        else:
            c_q, c_kv, k_r, q_mem = jnp.split(h @ mla_w_in[j], MLA_SPLITS, axis=-1)
            o = mla_attention(c_q, c_kv, k_r, mla_q_norm[j], mla_kv_norm[j], mla_w_uq[j], mla_w_ukv[j],
                              mla_qn_norm[j], mla_qr_norm[j], mla_kn_norm[j], mla_kr_norm[j], cos, sin)
        o_mem = memory_cross_attention(q_mem, rmsnorm(mem, mem_norm[i]), mem_w_kv[i],
                                       memq_norm[i], memk_norm[i])
        x = x + jnp.concatenate([o, o_mem], axis=-1) @ w_out[i]
        x = x + 0.5 * swiglu(rmsnorm(x, ffn2_norm[i]), ffn2_w_gate[i], ffn2_w_up[i], ffn2_w_down[i])
    return x
```

```python
import numpy as np
import concourse.bass as bass
import concourse.mybir as mybir
from concourse.bass_utils import run_bass_kernel_spmd
from contextlib import ExitStack

F32 = mybir.dt.float32
BF16 = mybir.dt.bfloat16
AF = mybir.ActivationFunctionType
ALU = mybir.AluOpType
AX = mybir.AxisListType

D = 1024
DFF = 2816
NF = DFF // 128
EPS = 1e-6


class Op:
    __slots__ = ("eng", "fn", "is_dma", "clock", "pos", "waits", "known", "inc", "count", "sem", "ndma")


class Prog:
    ENGS = ("pe", "act", "dve", "pool", "sp")

    def __init__(self, nc, es):
        self.nc = nc
        self.es = es
        self.sem = {e: es.enter_context(nc.semaphore("s_" + e)) for e in self.ENGS}
        self.cnt = {e: 0 for e in self.ENGS}
        self.dma_sem = {}
        self.dma_cnt = {}
        self.sem_pool = {}
        self.sem_used = {}
        self.n_ops = 0
        self._reset()

    def _reset(self):
        self.streams = {e: [] for e in self.ENGS}
        self.last_writer = {}
        self.readers = {}
        self.known = {e: {} for e in self.ENGS}
        self.dma_pos = {}
        self.blk_dma = {}

    def _record(self, op, reads, writes):
        eng = op.eng
        raw = set()
        deps = set()
        for k in reads:
            w = self.last_writer.get(k)
            if w is not None:
                deps.add(w)
                raw.add(w)
        for k in writes:
            w = self.last_writer.get(k)
            if w is not None:
                deps.add(w)
            for r in self.readers.get(k, {}).values():
                deps.add(r)
        kn = self.known[eng]
        op.waits = []
        for d in sorted(deps, key=lambda d: -d.pos):
            if d.clock == eng and eng == "pe":
                continue
            if kn.get(d.clock, 0) >= d.pos:
                continue
            op.waits.append(d)
            for c, p in d.known.items():
                if kn.get(c, 0) < p:
                    kn[c] = p
            if kn.get(d.clock, 0) < d.pos:
                kn[d.clock] = d.pos
        op.known = dict(kn)
        self.streams[eng].append(op)
        for k in reads:
            self.readers.setdefault(k, {})[op.clock] = op
        for k in writes:
            self.last_writer[k] = op
            self.readers[k] = {}
        self.n_ops += 1

    def add(self, eng, fn, reads=(), writes=()):
        op = Op()
        op.eng = eng
        op.fn = fn
        op.is_dma = False
        op.clock = eng
        op.pos = len(self.streams[eng]) + 1
        op.inc = False
        op.count = None
        op.sem = self.sem[eng]
        self._record(op, reads, writes)
        return op

    def dma(self, eng, key, pairs, reads=(), writes=(), **kw):
        self._dma_key(key, "sw" if eng == "pool" else "hw")
        op = Op()
        op.eng = eng
        op.is_dma = True
        op.clock = ("dma", key)
        self.dma_pos[key] = self.dma_pos.get(key, 0) + 1
        op.pos = self.dma_pos[key]
        op.inc = True
        op.ndma = len(pairs)
        self.dma_cnt[key] += 16 * len(pairs)
        op.count = self.dma_cnt[key]
        op.sem = self.dma_sem[key]
        sem = op.sem

        def fn(e, pairs=pairs, sem=sem, kw=kw):
            for (o, i) in pairs:
                e.dma_start(out=o, in_=i, **kw).then_inc(sem, 16)
        op.fn = fn
        self._record(op, reads, writes)
        self.blk_dma[key] = op
        return op

    def _dma_key(self, key, kind):
        if key not in self.dma_sem:
            pool = self.sem_pool.setdefault(kind, [])
            used = self.sem_used.get(kind, 0)
            if used == len(pool):
                pool.append([self.es.enter_context(self.nc.semaphore("d%s_%d" % (kind, len(pool)))), 0])
            ent = pool[used]
            self.sem_used[kind] = used + 1
            self.dma_sem[key] = ent[0]
            self.dma_cnt[key] = ent[1]
            self.key_ent = getattr(self, "key_ent", {})
            self.key_ent[key] = ent

    def emit(self):
        nc = self.nc
        for key, op in self.blk_dma.items():
            f = Op()
            f.eng = op.eng
            f.fn = None
            f.is_dma = False
            f.clock = op.eng
            f.pos = len(self.streams[op.eng]) + 1
            f.inc = False
            f.count = None
            f.sem = None
            f.waits = [op]
            f.known = {}
            self.streams[op.eng].append(f)
        for e in self.ENGS:
            for op in self.streams[e]:
                for t in op.waits:
                    t.inc = True
        for e in self.ENGS:
            c = self.cnt[e]
            for op in self.streams[e]:
                if op.is_dma or op.fn is None:
                    continue
                if op.inc:
                    c += 1
                    op.count = c
            self.cnt[e] = c
        reg = {"pe": "tensor", "act": "scalar", "dve": "vector", "pool": "gpsimd", "sp": "sync"}
        with nc.Block() as block:
            for e in self.ENGS:
                ops = self.streams[e]
                mysem = self.sem[e]

                def body(eng, ops=ops, mysem=mysem):
                    for op in ops:
                        for t in op.waits:
                            eng.wait_ge(t.sem, t.count)
                        if op.fn is None:
                            continue
                        if op.is_dma:
                            op.fn(eng)
                        else:
                            ins = op.fn(eng)
                            if op.inc:
                                ins.then_inc(mysem, 1)
                getattr(block, reg[e])(body)
        for key, ent in getattr(self, "key_ent", {}).items():
            ent[1] = self.dma_cnt[key]
        self.key_ent = {}
        self.dma_sem = {}
        self.dma_cnt = {}
        self.sem_used = {}
        self._reset()

    def begin(self):
        self.pes = ExitStack()
        self.nph = getattr(self, "nph", 0) + 1

    def end(self):
        self.emit()
        self.pes.close()

    def sb(self, name, shape, dtype):
        return self.pes.enter_context(self.nc.sbuf_tensor("sb%d_%s" % (self.nph, name), shape, dtype))

    def ps(self, name, shape, dtype):
        return self.pes.enter_context(self.nc.psum_tensor("ps%d_%s" % (self.nph, name), shape, dtype))

    def mm(self, out, pairs, reads, writes, **kw):
        n = len(pairs)

        def fn(e, out=out, pairs=pairs, n=n, kw=kw):
            ins = None
            for i, (l, r) in enumerate(pairs):
                ins = e.matmul(out, lhsT=l, rhs=r, start=(i == 0), stop=(i == n - 1), **kw)
            return ins
        return self.add("pe", fn, reads, writes)

    def mm1(self, out, lhsT, rhs, first, last, reads, writes):
        return self.add("pe", lambda e, o=out, l=lhsT, r=rhs, f=first, la=last:
                        e.matmul(o, lhsT=l, rhs=r, start=f, stop=la), reads, writes)

    def tr(self, out, in_, ident, reads, writes):
        return self.add("pe", lambda e, o=out, i=in_, d=ident: e.transpose(o, i, d), reads, writes)

    def actf(self, out, in_, func, reads, writes, eng="act", **kw):
        return self.add(eng, lambda e, o=out, i=in_, f=func, kw=kw: e.activation(out=o, in_=i, func=f, **kw),
                        reads, writes)

    def tt(self, eng, out, in0, in1, op, reads, writes):
        return self.add(eng, lambda e, o=out, a=in0, b=in1, op=op: e.tensor_tensor(out=o, in0=a, in1=b, op=op),
                        reads, writes)

    def ts(self, eng, out, in0, s1, s2, op0, op1, reads, writes):
        if op1 is None:
            return self.add(eng, lambda e, o=out, a=in0, s1=s1, op0=op0:
                            e.tensor_scalar(out=o, in0=a, scalar1=s1, scalar2=None, op0=op0), reads, writes)
        return self.add(eng, lambda e, o=out, a=in0, s1=s1, s2=s2, op0=op0, op1=op1:
                        e.tensor_scalar(out=o, in0=a, scalar1=s1, scalar2=s2, op0=op0, op1=op1), reads, writes)

    def stt(self, out, in0, scalar, in1, op0, op1, reads, writes):
        return self.add("dve", lambda e, o=out, a=in0, s=scalar, b=in1, op0=op0, op1=op1:
                        e.scalar_tensor_tensor(out=o, in0=a, scalar=s, in1=b, op0=op0, op1=op1), reads, writes)

    def copy(self, eng, out, in_, reads, writes):
        if eng == "act":
            return self.add(eng, lambda e, o=out, i=in_: e.copy(out=o, in_=i), reads, writes)
        return self.add(eng, lambda e, o=out, i=in_: e.tensor_copy(out=o, in_=i), reads, writes)


class Ctx:
    pass


def load_norm(P, C, src, blk, tag):
    for sub in range(4):
        t0 = blk * 512 + sub * 128
        xs = C.xs[sub % 2]
        kxs = ("xs", sub % 2)
        P.dma("sp", kxs, [(xs[:], src[t0:t0 + 128, :])], reads=[("dram", tag, blk)], writes=[kxs])
        P.add("act", lambda e, o=C.junk[:], i=xs[:], a=C.ss[:, sub:sub + 1]:
              e.activation(out=o, in_=i, func=AF.Square, accum_out=a), [kxs], ["junk", ("ss", sub)])
        P.ts("pool", C.sq[:, sub:sub + 1], C.ss[:, sub:sub + 1], float(D) * EPS, None, ALU.add, None,
             [("ss", sub)], [("sq", sub)])
        P.tt("pool", C.rstd[:, sub:sub + 1], C.sq[:, sub:sub + 1], C.mhalf[:, 0:1], ALU.pow,
             [("sq", sub), "mhalf"], [("rstd", sub)])
        P.ts("dve", C.xn[:, sub, :], xs[:], C.rstd[:, sub:sub + 1], float(D) ** 0.5, ALU.mult, ALU.mult,
             [kxs, ("rstd", sub)], [("xn", sub)])


def transpose_hT(P, C, gvec):
    for kt in range(8):
        pt = C.pt[kt % 2][:, 0:512]
        kpt = ("pt", kt % 2)
        for sub in range(4):
            P.tr(pt[:, sub * 128:(sub + 1) * 128], C.xn[:, sub, kt * 128:(kt + 1) * 128], C.ident[:],
                 [("xn", sub), "ident"], [kpt])
        P.ts("dve", C.hT[:, kt, :], pt, gvec[:, kt:kt + 1], None, ALU.mult, None, [kpt, "gvec"], [("hT", kt)])


def _cc(P, key, ins, outs, groups, reads, writes):
    P._dma_key(key, "cc")
    op = Op()
    op.eng = "pool"
    op.is_dma = True
    op.clock = ("dma", key)
    P.dma_pos[key] = P.dma_pos.get(key, 0) + 1
    op.pos = P.dma_pos[key]
    op.inc = True
    P.dma_cnt[key] += 1
    op.count = P.dma_cnt[key]
    op.sem = P.dma_sem[key]
    sem = op.sem

    def fn(e):
        e.collective_compute("AllGather", ALU.bypass, replica_groups=groups, ins=[ins], outs=[outs]).then_inc(sem)
    op.fn = fn
    P._record(op, reads, writes)
    P.blk_dma[key] = op


def alloc_xin(P, C, K, gnorm):
    C.xs = [P.sb("xs%d" % i, [128, D], F32) for i in range(2)]
    C.junk = P.sb("junk", [128, D], BF16)
    C.xn = P.sb("xn", [128, 4, D], BF16)
    C.hT = P.sb("hT", [128, 8, 512], BF16)
    C.gvec = P.sb("gvec", [128, 8], F32)
    C.ss = P.sb("ss", [128, 4], F32)
    C.sq = P.sb("sq", [128, 4], F32)
    C.rstd = P.sb("rstd", [128, 4], F32)
    C.mhalf = P.sb("mhalf", [128, 4], F32)
    C.ident = P.sb("ident", [128, 128], BF16)
    C.pt = [P.ps("pt%d" % i, [128, 1024], BF16) for i in range(2)]
    P.dma("pool", "ident", [(C.ident[:], K["ident"])], writes=["ident"])
    P.add("pool", lambda e: e.memset(C.mhalf[:], -0.5), [], ["mhalf"])
    P.dma("sp", "gvec", [(C.gvec[:], gnorm)], writes=["gvec"])


def load_w(P, key, dst, src_v, n0, n1, step=512):
    nk = src_v.shape[1]
    keys = []
    for lo in range(n0, n1, step):
        hi = min(n1, lo + step)
        k = (key, (lo - n0) // step)
        P.dma("pool", k, [(dst[:, kt, lo - n0:hi - n0], src_v[:, kt, lo:hi]) for kt in range(nk)], writes=[k])
        keys.append(k)
    return keys


def tok_view(ap, t0, nsub=4):
    return ap[t0:t0 + nsub * 128, :].rearrange("(s p) c -> p s c", p=128)


def emit_ffn(P, K, src, dst, wg, wu, wd, gnorm, T, tag_src="x", tag_dst="x"):
    P.begin()
    C = Ctx()
    nblk = T // 512
    alloc_xin(P, C, K, gnorm)
    C.wg = P.sb("wg", [128, 8, DFF], BF16)
    C.wu = P.sb("wu", [128, 8, DFF], BF16)
    C.wd = P.sb("wd", [128, NF, D], BF16)
    C.actT = P.sb("actT", [128, NF, 512], BF16)
    C.xres = P.sb("xres", [128, 4, D], F32)
    C.sg = [P.sb("sg%d" % i, [128, 512], F32) for i in range(2)]
    C.pg = [P.ps("pg%d" % i, [128, 512], F32) for i in range(2)]
    C.pu = [P.ps("pu%d" % i, [128, 512], F32) for i in range(2)]
    C.pd = [P.ps("pd%d" % i, [128, 512], F32) for i in range(2)]
    wg_v = wg.rearrange("(kt p) f -> p kt f", p=128)
    wu_v = wu.rearrange("(kt p) f -> p kt f", p=128)
    wd_v = wd.rearrange("(ft p) d -> p ft d", p=128)
    nch = (DFF + 511) // 512
    for c in range(nch):
        lo, hi = c * 512, min(DFF, c * 512 + 512)
        P.dma("pool", ("wg", c), [(C.wg[:, kt, lo:hi], wg_v[:, kt, lo:hi]) for kt in range(8)], writes=[("wg", c)])
        P.dma("pool", ("wu", c), [(C.wu[:, kt, lo:hi], wu_v[:, kt, lo:hi]) for kt in range(8)], writes=[("wu", c)])
    for c in range(6):
        fts = list(range(c * 4, min(NF, c * 4 + 4)))
        P.dma("pool", ("wd", c), [(C.wd[:, ft, :], wd_v[:, ft, :]) for ft in fts], writes=[("wd", c)])
    load_norm(P, C, src, 0, tag_src)
    transpose_hT(P, C, C.gvec)
    for blk in range(nblk):
        for f in range(NF):
            pg = C.pg[f % 2]
            pu = C.pu[f % 2]
            P.mm(pg[:], [(C.wg[:, kt, f * 128:(f + 1) * 128], C.hT[:, kt, :]) for kt in range(8)],
                 [("wg", f // 4)] + [("hT", kt) for kt in range(8)], [("pg", f % 2)])
            P.mm(pu[:], [(C.wu[:, kt, f * 128:(f + 1) * 128], C.hT[:, kt, :]) for kt in range(8)],
                 [("wu", f // 4)] + [("hT", kt) for kt in range(8)], [("pu", f % 2)])
            sg = C.sg[f % 2]
            P.actf(sg[:], pg[:], AF.Silu, [("pg", f % 2)], [("sg", f % 2)])
            P.tt("dve", C.actT[:, f, :], sg[:], pu[:], ALU.mult, [("sg", f % 2), ("pu", f % 2)], [("actT", f)])
            if f == 6 and blk + 1 < nblk:
                load_norm(P, C, src, blk + 1, tag_src)
        t0 = blk * 512
        P.dma("sp", "xres", [(C.xres[:], tok_view(src, t0))], reads=[("dram", tag_src, blk)], writes=["xres"])
        if blk + 1 < nblk:
            transpose_hT(P, C, C.gvec)
        for sub in range(4):
            for dh in range(2):
                pd = C.pd[(sub * 2 + dh) % 2]
                kpd = ("pd", (sub * 2 + dh) % 2)
                P.mm(pd[:], [(C.actT[:, f, sub * 128:(sub + 1) * 128], C.wd[:, f, dh * 512:(dh + 1) * 512])
                             for f in range(NF)],
                     [("wd", c) for c in range(6)] + [("actT", f) for f in range(NF)], [kpd])
                xr = C.xres[:, sub, dh * 512:(dh + 1) * 512]
                P.stt(xr, pd[:], 0.5, xr, ALU.mult, ALU.add, [kpd, "xres"], ["xres"])
        P.dma("pool", "xst", [(tok_view(dst, t0), C.xres[:])], reads=["xres"], writes=[("dram", tag_dst, blk)])
    P.end()


def emit_outproj(P, K, X, OT, wout, T):
    P.begin()
    nblk = T // 512
    wo = P.sb("wo", [128, 12, D], BF16)
    ot = [P.sb("ot%d" % i, [128, 12, 512], BF16) for i in range(2)]
    xres = P.sb("xres", [128, 4, D], F32)
    pd = [P.ps("pd%d" % i, [128, 512], F32) for i in range(4)]
    wo_v = wout.rearrange("(ft p) d -> p ft d", p=128)
    for c in range(3):
        P.dma("pool", ("wo", c), [(wo[:, ft, :], wo_v[:, ft, :]) for ft in range(c * 4, c * 4 + 4)], writes=[("wo", c)])
    OT_v = OT.rearrange("(ft p) t -> p ft t", p=128)
    for blk in range(nblk):
        t0 = blk * 512
        o = ot[blk % 2]
        ko = ("ot", blk % 2)
        P.dma("sp", ko, [(o[:], OT_v[:, :, t0:t0 + 512])], writes=[ko])
        P.dma("sp", "xres", [(xres[:], tok_view(X, t0))], writes=["xres"])
        for sub in range(4):
            for dh in range(2):
                i = (sub * 2 + dh) % 4
                P.mm(pd[i][:], [(o[:, ft, sub * 128:(sub + 1) * 128], wo[:, ft, dh * 512:(dh + 1) * 512])
                                for ft in range(12)], [ko] + [("wo", c) for c in range(3)], [("pd", i)])
                xr = xres[:, sub, dh * 512:(dh + 1) * 512]
                P.tt("dve", xr, pd[i][:], xr, ALU.add, [("pd", i), "xres"], ["xres"])
        P.dma("pool", "xst", [(tok_view(X, t0), xres[:])], reads=["xres"])
    P.end()


def alloc_fnorm(P, C):
    C.sqb = [P.sb("sqb%d" % i, [128, 512], BF16) for i in range(3)]
    C.lnb = P.sb("lnb", [128, 512], F32)
    C.rsb = P.sb("rsb", [128, 512], F32)
    C.ones = P.sb("ones", [128, 128], BF16)
    C.epsT = P.sb("epsT", [128, 1], F32)
    C.oneT = P.sb("oneT", [128, 1], F32)
    C.pss = P.ps("pss", [128, 512], F32)
    P.add("pool", lambda e: e.memset(C.ones[:], 1.0), [], ["ones"])
    P.add("pool", lambda e: e.memset(C.epsT[:], EPS), [], ["epsT"])
    P.add("pool", lambda e: e.memset(C.oneT[:], 1.0), [], ["oneT"])


def fnorm(P, C, srcs, np_, nfeat, ncol, outs, extra_reads=()):
    n = len(srcs)
    for i, (src, k) in enumerate(srcs):
        P.actf(C.sqb[i][0:np_, 0:ncol], src, AF.Square, [k], [("sqb", i)])
    P.mm(C.pss[0:np_, 0:ncol], [(C.ones[0:np_, 0:np_], C.sqb[i][0:np_, 0:ncol]) for i in range(n)],
         [("sqb", i) for i in range(n)] + ["ones"], ["pss"])
    P.actf(C.lnb[0:np_, 0:ncol], C.pss[0:np_, 0:ncol], AF.Ln, ["pss", "epsT"], ["lnb"],
           scale=1.0 / nfeat, bias=C.epsT[0:np_, 0:1])
    P.actf(C.rsb[0:np_, 0:ncol], C.lnb[0:np_, 0:ncol], AF.Exp, ["lnb"], ["rsb"], scale=-0.5)
    for (o, g, s, sk, ok) in outs:
        P.stt(o, s, g, C.rsb[0:np_, 0:ncol], ALU.mult, ALU.mult, [sk, "rsb"] + list(extra_reads), [ok])


def emit_mem(P, K, X, OT, wq_v, ncol0, gmix, mem, gmem, wkv, gq, gk, T):
    P.begin()
    C = Ctx()
    nblk = T // 512
    alloc_xin(P, C, K, gmix)
    alloc_fnorm(P, C)
    wq = P.sb("wq", [128, 8, 512], BF16)
    wk = P.sb("wkv", [128, 8, 1024], BF16)
    gm = P.sb("gm", [128, 8], F32)
    gqs = P.sb("gqs", [128, 1], F32)
    gks = P.sb("gks", [128, 1], F32)
    memT = P.sb("memT", [128, 8, 256], BF16)
    KmT = P.sb("KmT", [128, 4, 256], BF16)
    Vm = P.sb("Vm", [128, 2, 512], BF16)
    qmn = [P.sb("qmn%d" % i, [128, 512], BF16) for i in range(2)]
    PT = [P.sb("PT%d" % i, [128, 2, 512], BF16) for i in range(2)]
    rL = P.sb("rL", [128, 512], F32)
    omT = [P.sb("omT%d" % i, [128, 4, 512], BF16) for i in range(2)]
    pq = [P.ps("pq0", [128, 512], F32)] * 2
    psT = [P.ps("psT%d" % i, [128, 512], F32) for i in range(2)]
    po = P.ps("po", [128, 512], F32)
    pL = P.ps("pL", [128, 512], F32)
    kq = load_w(P, "wq", wq, wq_v, ncol0, ncol0 + 512)
    kk = load_w(P, "wkv", wk, wkv.rearrange("(kt p) f -> p kt f", p=128), 0, 1024)
    P.dma("sp", "gm", [(gm[:], gmem)], writes=["gm"])
    P.dma("sp", "gqs", [(gqs[:], gq)], writes=["gqs"])
    P.dma("sp", "gks", [(gks[:], gk)], writes=["gks"])
    for sub in range(2):
        xs = C.xs[sub]
        kxs = ("xs", sub)
        P.dma("sp", kxs, [(xs[:], mem[sub * 128:(sub + 1) * 128, :])], writes=[kxs])
        P.add("act", lambda e, o=C.junk[:], i=xs[:], a=C.ss[:, sub:sub + 1]:
              e.activation(out=o, in_=i, func=AF.Square, accum_out=a), [kxs], ["junk", ("ss", sub)])
        P.ts("pool", C.sq[:, sub:sub + 1], C.ss[:, sub:sub + 1], float(D) * EPS, None, ALU.add, None,
             [("ss", sub)], [("sq", sub)])
        P.tt("pool", C.rstd[:, sub:sub + 1], C.sq[:, sub:sub + 1], C.mhalf[:, 0:1], ALU.pow,
             [("sq", sub), "mhalf"], [("rstd", sub)])
        P.ts("dve", C.xn[:, sub, :], xs[:], C.rstd[:, sub:sub + 1], float(D) ** 0.5, ALU.mult, ALU.mult,
             [kxs, ("rstd", sub)], [("xn", sub)])
    for kt in range(8):
        pt = C.pt[kt % 2][:, 0:256]
        kpt = ("pt", kt % 2)
        for sub in range(2):
            P.tr(pt[:, sub * 128:(sub + 1) * 128], C.xn[:, sub, kt * 128:(kt + 1) * 128], C.ident[:],
                 [("xn", sub), "ident"], [kpt])
        P.ts("dve", memT[:, kt, :], pt, gm[:, kt:kt + 1], None, ALU.mult, None, [kpt, "gm"], [("memT", kt)])
    rmem = [("memT", kt) for kt in range(8)]
    for hd in range(4):
        p = pq[hd % 2]
        kp = ("pq", 0)
        P.mm(p[:, 0:256], [(wk[:, kt, hd * 128:(hd + 1) * 128], memT[:, kt, :]) for kt in range(8)], rmem + kk, [kp])
        fnorm(P, C, [(p[:, 0:256], kp)], 128, 128.0, 256, [(KmT[:, hd, :], gks[:, 0:1], p[:, 0:256], kp, ("KmT", hd))],
              ["gks"])
    for mt in range(2):
        p = pq[mt % 2]
        kp = ("pq", 0)
        P.mm(p[:], [(memT[:, kt, mt * 128:(mt + 1) * 128], wk[:, kt, 512:1024]) for kt in range(8)], rmem + kk, [kp])
        P.copy("act", Vm[:, mt, :], p[:], [kp], [("Vm", mt)])
    OT_v = OT[1024:1536, :].rearrange("(h p) t -> p h t", p=128)
    scale = 128.0 ** -0.5
    load_norm(P, C, X, 0, "x")
    for blk in range(nblk):
        transpose_hT(P, C, C.gvec)
        if blk + 1 < nblk:
            load_norm(P, C, X, blk + 1, "x")
        om = omT[blk % 2]
        for hd in range(4):
            p = pq[hd % 2]
            kp = ("pq", 0)
            q = qmn[hd % 2]
            kqn = ("qmn", hd % 2)
            P.mm(p[:], [(wq[:, kt, hd * 128:(hd + 1) * 128], C.hT[:, kt, :]) for kt in range(8)],
                 [("hT", kt) for kt in range(8)] + kq, [kp])
            fnorm(P, C, [(p[:], kp)], 128, 128.0, 512, [(q[:], gqs[:, 0:1], p[:], kp, kqn)], ["gqs"])
            pt_ = PT[hd % 2]
            for mt in range(2):
                P.mm(psT[mt][:], [(KmT[:, hd, mt * 128:(mt + 1) * 128], q[:])], [("KmT", hd), kqn], [("psT", mt)])
                P.actf(pt_[:, mt, :], psT[mt][:], AF.Exp, [("psT", mt)], [("PT", hd % 2, mt)], scale=scale)
            rpt = [("PT", hd % 2, mt) for mt in range(2)]
            P.mm(po[:], [(Vm[:, mt, hd * 128:(hd + 1) * 128], pt_[:, mt, :]) for mt in range(2)],
                 rpt + [("Vm", 0), ("Vm", 1)], ["po"])
            P.mm(pL[:], [(C.ones[:, :], pt_[:, mt, :]) for mt in range(2)], rpt + ["ones"], ["pL"])
            P.add("dve", lambda e, o=rL[:], i=pL[:]: e.reciprocal(out=o, in_=i), ["pL"], ["rL"])
            P.tt("dve", om[:, hd, :], po[:], rL[:], ALU.mult, ["po", "rL"], [("om", blk % 2)])
        P.dma("pool", ("omst", blk % 2), [(OT_v[:, :, blk * 512:(blk + 1) * 512], om[:])], reads=[("om", blk % 2)])
    P.end()


GQ, GK_, GV, GA, GR = 0, 512, 1024, 2048, 2064


def emit_gla_proj(P, K, X, win, gmix, walpha, balpha_b, gout_b, S, T):
    P.begin()
    C = Ctx()
    nblk = T // 512
    NCH = T // 128
    alloc_xin(P, C, K, gmix)
    w = P.sb("w", [128, 8, 3088], BF16)
    wal = P.sb("wal", [16, 512], BF16)
    bal = P.sb("bal", [128, 512], F32)
    gob = P.sb("gob", [128, 1024], F32)
    R1 = P.sb("R1", [128, 128], F32)
    TRI = P.sb("TRI", [128, 128], F32)
    M3 = P.sb("M3", [128, 128], F32)
    oneT = P.sb("oneT", [128, 1], F32)
    qT = P.sb("qT", [128, 4, 512], F32)
    kT = P.sb("kT", [128, 4, 512], F32)
    ktok = P.sb("ktok", [128, 4, 512], F32)
    vsb = P.sb("vsb", [128, 4, 1024], BF16)
    alT = P.sb("alT", [16, 512], BF16)
    zb = P.sb("zb", [128, 512], F32)
    la = P.sb("la", [128, 4, 512], F32)
    E = [P.sb("E%d" % i, [128, 512], F32) for i in range(4)]
    QpT = P.sb("QpT", [128, 4, 512], BF16)
    KpT = P.sb("KpT", [128, 4, 512], BF16)
    QppT = P.sb("QppT", [128, 4, 512], BF16)
    Kpp = P.sb("Kpp", [128, 4, 512], BF16)
    DEC = P.sb("DEC", [128, 4, NCH], F32)
    Rg = P.sb("Rg", [128, 4, 1024], BF16)
    sil = P.sb("sil", [128, 512], F32)
    bk = [P.ps("bk%d" % i, [128, 512], F32) for i in range(6)]
    win_v = win.rearrange("(kt p) f -> p kt f", p=128)
    kw = load_w(P, "w", w, win_v, 0, 3088)
    P.dma("pool", "wal", [(wal[:], walpha)], writes=["wal"])
    P.dma("sp", "bal", [(bal[:], balpha_b)], writes=["bal"])
    P.dma("sp", "gob", [(gob[:], gout_b)], writes=["gob"])
    P.dma("sp", "R1", [(R1[:], K["R1"])], writes=["R1"])
    P.dma("sp", "TRI", [(TRI[:], K["TRI"])], writes=["TRI"])
    P.dma("sp", "M3", [(M3[:], K["M3"])], writes=["M3"])
    P.add("pool", lambda e: e.memset(oneT[:], 1.0), [], ["oneT"])
    rh = [("hT", kt) for kt in range(8)]
    nb = [0]

    def bank():
        i = nb[0] % 6
        nb[0] += 1
        return bk[i], ("bk", i)

    load_norm(P, C, X, 0, "x")
    for blk in range(nblk):
        t0 = blk * 512
        transpose_hT(P, C, C.gvec)
        if blk + 1 < nblk:
            load_norm(P, C, X, blk + 1, "x")
        for hd in range(4):
            b, kb = bank()
            P.mm(b[:], [(w[:, kt, GQ + hd * 128:GQ + (hd + 1) * 128], C.hT[:, kt, :]) for kt in range(8)], rh + kw, [kb])
            P.actf(qT[:, hd, :], b[:], AF.Identity, [kb], [("qT", hd)], scale=128.0 ** -0.5)
            b, kb = bank()
            P.mm(b[:], [(w[:, kt, GK_ + hd * 128:GK_ + (hd + 1) * 128], C.hT[:, kt, :]) for kt in range(8)], rh + kw, [kb])
            P.copy("dve", kT[:, hd, :], b[:], [kb], [("kT", hd)])
        b, kb = bank()
        P.mm(b[0:16, :], [(w[:, kt, GA:GA + 16], C.hT[:, kt, :]) for kt in range(8)], rh + kw, [kb])
        P.copy("dve", alT[:], b[0:16, :], [kb], ["alT"])
        for sub in range(4):
            ts_ = slice(sub * 128, (sub + 1) * 128)
            b, kb = bank()
            P.mm(b[:], [(C.hT[:, kt, ts_], w[:, kt, GK_:GK_ + 512]) for kt in range(8)], rh + kw, [kb])
            P.copy("act", ktok[:, sub, :], b[:], [kb], [("ktok", sub)])
            for hf in range(2):
                b, kb = bank()
                P.mm(b[:], [(C.hT[:, kt, ts_], w[:, kt, GV + hf * 512:GV + (hf + 1) * 512]) for kt in range(8)],
                     rh + kw, [kb])
                P.copy("dve", vsb[:, sub, hf * 512:(hf + 1) * 512], b[:], [kb], [("vsb", sub)])
            b, kb = bank()
            P.mm(b[:], [(alT[0:16, ts_], wal[0:16, :])], ["alT", "wal"], [kb])
            P.tt("dve", zb[:], b[:], bal[:], ALU.add, [kb, "bal"], ["zb"])
            P.actf(zb[:], zb[:], AF.Exp, ["zb"], ["zb"], scale=-1.0)
            P.actf(zb[:], zb[:], AF.Ln, ["zb", "oneT"], ["zb"], bias=oneT[:, 0:1])
            P.ts("dve", la[:, sub, :], zb[:], -1.0 / 16.0, None, ALU.mult, None, ["zb"], [("la", sub)])
            bA1, kA1 = bank()
            bA2, kA2 = bank()
            bA3, kA3 = bank()
            for hd in range(4):
                hs = slice(hd * 128, (hd + 1) * 128)
                P.mm(bA1[:, hs], [(la[:, sub, hs], R1[:, :])], [("la", sub), "R1"], [kA1])
                P.mm(bA2[:, hs], [(la[:, sub, hs], TRI[:, :])], [("la", sub), "TRI"], [kA2])
            P.mm(bA3[:], [(M3[:, :], la[:, sub, :])], [("la", sub), "M3"], [kA3])
            P.actf(E[0][:], bA1[:], AF.Exp, [kA1], [("E", 0)])
            P.actf(E[1][:], bA1[:], AF.Exp, [kA1], [("E", 1)], scale=-1.0)
            P.actf(E[2][:], bA2[:], AF.Exp, [kA2], [("E", 2)])
            P.actf(E[3][:], bA3[:], AF.Exp, [kA3], [("E", 3)])
            e3 = lambda t: t[:].rearrange("p (h i) -> p h i", h=4)
            rq = [("qT", h) for h in range(4)]
            rk = [("kT", h) for h in range(4)]
            P.tt("dve", QpT[:, :, ts_], qT[:, :, ts_], e3(E[0]), ALU.mult, rq + [("E", 0)], [("QpT", sub)])
            P.tt("dve", KpT[:, :, ts_], kT[:, :, ts_], e3(E[1]), ALU.mult, rk + [("E", 1)], [("KpT", sub)])
            P.tt("dve", QppT[:, :, ts_], qT[:, :, ts_], e3(E[2]), ALU.mult, rq + [("E", 2)], [("QppT", sub)])
            P.tt("dve", Kpp[:, sub, :], ktok[:, sub, :], E[3][:], ALU.mult, [("ktok", sub), ("E", 3)], [("Kpp", sub)])
            P.copy("act", DEC[:, :, blk * 4 + sub], e3(E[2])[:, :, 127], [("E", 2)], ["DEC"])
            for hf in range(2):
                b, kb = bank()
                P.mm(b[:], [(C.hT[:, kt, ts_], w[:, kt, GR + hf * 512:GR + (hf + 1) * 512]) for kt in range(8)],
                     rh + kw, [kb])
                P.actf(sil[:], b[:], AF.Silu, [kb], ["sil"])
                P.stt(Rg[:, sub, hf * 512:(hf + 1) * 512], sil[:], 16.0, gob[:, hf * 512:(hf + 1) * 512],
                      ALU.mult, ALU.mult, ["sil", "gob"], [("Rg", sub)])
        fm = lambda ap: ap.rearrange("(h p) t -> p h t", p=128)[:, :, t0:t0 + 512]
        P.dma("pool", "stQ", [(fm(S["QpT"]), QpT[:]), (fm(S["KpT"]), KpT[:]), (fm(S["QppT"]), QppT[:])],
              reads=[(n, s_) for n in ("QpT", "KpT", "QppT") for s_ in range(4)])
        P.dma("pool", "stK", [(tok_view(S["Kpp"], t0), Kpp[:]), (tok_view(S["V"], t0), vsb[:]),
                              (tok_view(S["Rg"], t0), Rg[:])],
              reads=[(n, s_) for n in ("Kpp", "vsb", "Rg") for s_ in range(4)])
    P.dma("pool", "stD", [(S["DEC"], DEC[:].rearrange("p h c -> p (h c)"))], reads=["DEC"])
    P.end()


def emit_gla_scan(P, K, S, OT, T, with_out, Sbounce=None, Sgath=None, flag=None):
    P.begin()
    NCH = T // 128
    ngrp = NCH // 4
    St = P.sb("St", [128, 4, 256], F32)
    Sb = P.sb("Sb", [128, 4, 256], BF16)
    DEC = P.sb("DEC", [128, 4, NCH], F32)
    Kpp = [P.sb("Kpp%d" % i, [128, 4, 512], BF16) for i in range(2)]
    V = [P.sb("V%d" % i, [128, 4, 1024], BF16) for i in range(2)]
    pu = [P.ps("pu%d" % i, [128, 512], F32) for i in range(2)]
    P.dma("sp", "DEC", [(DEC[:].rearrange("p h c -> p (h c)"), S["DEC"])], writes=["DEC"])
    if with_out:
        QpT = [P.sb("QpT%d" % i, [128, 4, 512], BF16) for i in range(2)]
        KpT = [P.sb("KpT%d" % i, [128, 4, 512], BF16) for i in range(2)]
        QppT = [P.sb("QppT%d" % i, [128, 4, 512], BF16) for i in range(2)]
        Rg = [P.sb("Rg%d" % i, [128, 4, 1024], BF16) for i in range(2)]
        maskT = P.sb("maskT", [128, 128], F32)
        scm = [P.sb("scm%d" % i, [128, 128], BF16) for i in range(2)]
        og = [P.sb("og%d" % i, [128, 256], BF16) for i in range(2)]
        oT = [P.sb("oT%d" % i, [128, 8, 512], BF16) for i in range(2)]
        junk = P.sb("junk", [128, 256], BF16)
        ss = P.sb("ss", [128, 8], F32)
        sq = P.sb("sq", [128, 8], F32)
        rs = P.sb("rs", [128, 8], F32)
        mhalf = P.sb("mhalf", [128, 1], F32)
        flg = P.sb("flg", [128, 1], F32)
        ident = P.sb("ident", [128, 128], BF16)
        psc = [P.ps("psc%d" % i, [128, 512], F32) for i in range(2)]
        po = [P.ps("po%d" % i, [128, 512], F32) for i in range(2)]
        ptr = [P.ps("ptr%d" % i, [128, 1024], BF16) for i in range(2)]
        P.dma("sp", "maskT", [(maskT[:], K["TRI"])], writes=["maskT"])
        P.dma("pool", "ident", [(ident[:], K["ident"])], writes=["ident"])
        P.add("pool", lambda e: e.memset(mhalf[:], -0.5), [], ["mhalf"])
        P.dma("sp", "flg", [(flg[:], flag)], writes=["flg"])
        P.dma("sp", "St", [(St[:].rearrange("p h v -> p (h v)"), Sgath[0:128, :])], writes=["St"])
        P.ts("dve", St[:], St[:], flg[:, 0:1], None, ALU.mult, None, ["St", "flg"], [("S", h) for h in range(4)])
        for h in range(4):
            P.copy("act", Sb[:, h, :], St[:, h, :], [("S", h)], [("Sb", h)])
    else:
        P.add("dve", lambda e: e.memset(St[:], 0.0), [], [("S", h) for h in range(4)])
    fm = lambda ap, g: ap.rearrange("(h p) t -> p h t", p=128)[:, :, g * 512:(g + 1) * 512]
    OT_v = OT[0:1024, :].rearrange("(f p) t -> p f t", p=128) if with_out else None
    n = 0
    for g in range(ngrp):
        b2 = g % 2
        t0 = g * 512
        P.dma("sp", ("ldK", b2), [(Kpp[b2][:], tok_view(S["Kpp"], t0)), (V[b2][:], tok_view(S["V"], t0))],
              writes=[("Kpp", b2), ("V", b2)])
        if with_out:
            P.dma("sp", ("ldQ", b2), [(QpT[b2][:], fm(S["QpT"], g)), (KpT[b2][:], fm(S["KpT"], g)),
                                      (QppT[b2][:], fm(S["QppT"], g)), (Rg[b2][:], tok_view(S["Rg"], t0))],
                  writes=[("Q", b2)])
        for cc in range(4):
            c = g * 4 + cc
            ts_ = slice(cc * 128, (cc + 1) * 128)
            for hd in range(4):
                i2 = n % 2
                n += 1
                hs = slice(hd * 128, (hd + 1) * 128)
                vs = slice(hd * 256, (hd + 1) * 256)
                if with_out:
                    P.mm(psc[i2][:, 0:128], [(KpT[b2][:, hd, ts_], QpT[b2][:, hd, ts_])], [("Q", b2)], [("psc", i2)])
                    P.tt("dve", scm[i2][:], psc[i2][:, 0:128], maskT[:], ALU.mult, [("psc", i2), "maskT"], [("scm", i2)])
                    P.mm(po[i2][:, 0:256], [(QppT[b2][:, hd, ts_], Sb[:, hd, :]), (scm[i2][:], V[b2][:, cc, vs])],
                         [("Q", b2), ("Sb", hd), ("scm", i2), ("V", b2)], [("po", i2)])
                P.mm(pu[i2][:, 0:256], [(Kpp[b2][:, cc, hs], V[b2][:, cc, vs])], [("Kpp", b2), ("V", b2)], [("pu", i2)])
                P.stt(St[:, hd, :], St[:, hd, :], DEC[:, hd, c:c + 1], pu[i2][:, 0:256], ALU.mult, ALU.add,
                      [("S", hd), "DEC", ("pu", i2)], [("S", hd)])
                if with_out:
                    P.copy("act", Sb[:, hd, :], St[:, hd, :], [("S", hd)], [("Sb", hd)])
                    k8 = n % 8
                    P.add("act", lambda e, o=junk[:], i=po[i2][:, 0:256], a=ss[:, k8:k8 + 1]:
                          e.activation(out=o, in_=i, func=AF.Square, accum_out=a), [("po", i2)], ["junk", ("ss", k8)])
                    P.ts("pool", sq[:, k8:k8 + 1], ss[:, k8:k8 + 1], 256.0 * EPS, None, ALU.add, None,
                         [("ss", k8)], [("sq", k8)])
                    P.tt("pool", rs[:, k8:k8 + 1], sq[:, k8:k8 + 1], mhalf[:, 0:1], ALU.pow,
                         [("sq", k8), "mhalf"], [("rs", k8)])
                    P.stt(og[i2][:], po[i2][:, 0:256], rs[:, k8:k8 + 1], Rg[b2][:, cc, vs], ALU.mult, ALU.mult,
                          [("po", i2), ("rs", k8), ("Q", b2)], [("og", i2)])
                    for vh in range(2):
                        P.tr(ptr[i2][:, vh * 128:(vh + 1) * 128], og[i2][:, vh * 128:(vh + 1) * 128], ident[:],
                             [("og", i2), "ident"], [("ptr", i2)])
                    P.copy("pool" if False else "dve", oT[b2][:, hd * 2:hd * 2 + 2, ts_],
                           ptr[i2][:, 0:256].rearrange("p (v i) -> p v i", v=2), [("ptr", i2)], [("oT", b2)])
        if with_out:
            P.dma("pool", ("stO", b2), [(OT_v[:, :, t0:t0 + 512], oT[b2][:])], reads=[("oT", b2)])
    if not with_out:
        P.dma("pool", "stS", [(Sbounce, St[:].rearrange("p h v -> p (h v)"))], reads=[("S", h) for h in range(4)])
    P.end()


def emit_mla_proj(P, K, X, win, gmix, gq384, gkv256, wuq, wukv, gqn, gqr2, gkn, gkr2, cosT, sinT, S, T):
    P.begin()
    C = Ctx()
    nblk = T // 512
    alloc_xin(P, C, K, gmix)
    alloc_fnorm(P, C)
    w = P.sb("w", [128, 8, 768], BF16)
    uq = P.sb("uq", [128, 3, 2048], BF16)
    ukn = P.sb("ukn", [128, 2, 1024], BF16)
    ukv = P.sb("ukv", [128, 2, 1024], BF16)
    g3 = P.sb("g3", [128, 3], F32)
    g2 = P.sb("g2", [128, 2], F32)
    gqn_s = P.sb("gqn", [128, 1], F32)
    gkn_s = P.sb("gkn", [128, 1], F32)
    gqr_s = P.sb("gqr", [64, 2], F32)
    gkr_s = P.sb("gkr", [64, 2], F32)
    cs = [P.sb("cs%d" % i, [64, 512], F32) for i in range(2)]
    sn = [P.sb("sn%d" % i, [64, 512], F32) for i in range(2)]
    cqn = P.sb("cqn", [128, 3, 512], BF16)
    ckn = P.sb("ckn", [128, 2, 512], BF16)
    ra = P.sb("ra", [64, 512], F32)
    rb = P.sb("rb", [64, 512], F32)
    QT = [P.sb("QT%d" % i, [128, 8, 512], BF16) for i in range(2)]
    QR = [P.sb("QR%d" % i, [64, 8, 512], BF16) for i in range(2)]
    KT = [P.sb("KT%d" % i, [128, 8, 512], BF16) for i in range(2)]
    KR = [P.sb("KR%d" % i, [64, 512], BF16) for i in range(2)]
    Vs = [P.sb("Vs%d" % i, [128, 4, 1024], BF16) for i in range(2)]
    bk = [P.ps("bk%d" % i, [128, 512], F32) for i in range(5)]
    win_v = win.rearrange("(kt p) f -> p kt f", p=128)
    kw = load_w(P, "w", w[:, :, 0:704], win_v, 0, 704)
    P.dma("pool", ("w", 9), [(w[:, kt, 704:736], win_v[:, kt, 672:704]) for kt in range(8)]
          + [(w[:, kt, 736:768], win_v[:, kt, 640:672]) for kt in range(8)], writes=[("w", 9)])
    kw.append(("w", 9))
    uq_v = wuq.rearrange("(ct p) (h f) -> p ct h f", p=128, h=8)
    uq_d = uq[:].rearrange("p ct (h f) -> p ct h f", h=8)
    P.dma("pool", "uq", [(uq_d[:, ct, :, 0:192], uq_v[:, ct, :, :]) for ct in range(3)]
          + [(uq_d[:, ct, :, 192:224], uq_v[:, ct, :, 160:192]) for ct in range(3)]
          + [(uq_d[:, ct, :, 224:256], uq_v[:, ct, :, 128:160]) for ct in range(3)], writes=["uq"])
    ukv_v = wukv.rearrange("(ct p) (h f) -> p ct h f", p=128, h=8)
    P.dma("pool", "ukv", [(ukn[:, ct, :].rearrange("p (h f) -> p h f", h=8), ukv_v[:, ct, :, 0:128]) for ct in range(2)]
          + [(ukv[:, ct, :].rearrange("p (h f) -> p h f", h=8), ukv_v[:, ct, :, 128:256]) for ct in range(2)],
          writes=["ukv"])
    for (t, src, k) in ((g3, gq384, "g3"), (g2, gkv256, "g2"), (gqn_s, gqn, "gqn"), (gkn_s, gkn, "gkn"),
                        (gqr_s, gqr2, "gqr"), (gkr_s, gkr2, "gkr")):
        P.dma("sp", k, [(t[:], src)], writes=[k])
    rh = [("hT", kt) for kt in range(8)]
    nb = [0]

    def bank():
        i = nb[0] % 5
        nb[0] += 1
        return bk[i], ("bk", i)

    def rope(src_a, ka, src_b, kb_, g2col, out, kout, b2, gkey):
        fnorm(P, C, [(src_a, ka)], 64, 64.0, 512,
              [(ra[:], g2col[:, 0:1], src_a, ka, "ra"), (rb[:], g2col[:, 1:2], src_b, kb_, "rb")], [gkey])
        P.tt("dve", ra[:], ra[:], cs[b2][:], ALU.mult, ["ra", ("cs", b2)], ["ra"])
        P.tt("dve", rb[:], rb[:], sn[b2][:], ALU.mult, ["rb", ("cs", b2)], ["rb"])
        P.tt("dve", out, ra[:], rb[:], ALU.add, ["ra", "rb"], [kout])

    load_norm(P, C, X, 0, "x")
    for blk in range(nblk):
        t0 = blk * 512
        b2 = blk % 2
        transpose_hT(P, C, C.gvec)
        if blk + 1 < nblk:
            load_norm(P, C, X, blk + 1, "x")
        P.dma("sp", ("cs", b2), [(cs[b2][:], cosT[:, t0:t0 + 512]), (sn[b2][:], sinT[:, t0:t0 + 512])],
              writes=[("cs", b2)])
        srcs = []
        for ct in range(3):
            b, kb = bank()
            P.mm(b[:], [(w[:, kt, ct * 128:(ct + 1) * 128], C.hT[:, kt, :]) for kt in range(8)], rh + kw, [kb])
            srcs.append((b[:], kb))
        fnorm(P, C, srcs, 128, 384.0, 512,
              [(cqn[:, ct, :], g3[:, ct:ct + 1], srcs[ct][0], srcs[ct][1], ("cqn", ct)) for ct in range(3)], ["g3"])
        srcs = []
        for ct in range(2):
            b, kb = bank()
            P.mm(b[:], [(w[:, kt, 384 + ct * 128:384 + (ct + 1) * 128], C.hT[:, kt, :]) for kt in range(8)], rh + kw, [kb])
            srcs.append((b[:], kb))
        fnorm(P, C, srcs, 128, 256.0, 512,
              [(ckn[:, ct, :], g2[:, ct:ct + 1], srcs[ct][0], srcs[ct][1], ("ckn", ct)) for ct in range(2)], ["g2"])
        ba, ka = bank()
        P.mm(ba[0:64, :], [(w[:, kt, 640:704], C.hT[:, kt, :]) for kt in range(8)], rh + kw, [ka])
        bb, kbb = bank()
        P.mm(bb[0:64, :], [(w[:, kt, 704:768], C.hT[:, kt, :]) for kt in range(8)], rh + kw, [kbb])
        rope(ba[0:64, :], ka, bb[0:64, :], kbb, gkr_s, KR[b2][:], ("KR", b2), b2, "gkr")
        rcq = [("cqn", ct) for ct in range(3)] + ["uq"]
        rck = [("ckn", ct) for ct in range(2)] + ["ukv"]
        for hd in range(8):
            o = hd * 256
            b, kb = bank()
            P.mm(b[:], [(uq[:, ct, o:o + 128], cqn[:, ct, :]) for ct in range(3)], rcq, [kb])
            fnorm(P, C, [(b[:], kb)], 128, 128.0, 512, [(QT[b2][:, hd, :], gqn_s[:, 0:1], b[:], kb, ("QT", b2))], ["gqn"])
            ba, ka = bank()
            P.mm(ba[0:64, :], [(uq[:, ct, o + 128:o + 192], cqn[:, ct, :]) for ct in range(3)], rcq, [ka])
            bb, kbb = bank()
            P.mm(bb[0:64, :], [(uq[:, ct, o + 192:o + 256], cqn[:, ct, :]) for ct in range(3)], rcq, [kbb])
            rope(ba[0:64, :], ka, bb[0:64, :], kbb, gqr_s, QR[b2][:, hd, :], ("QR", b2), b2, "gqr")
            b, kb = bank()
            P.mm(b[:], [(ukn[:, ct, hd * 128:(hd + 1) * 128], ckn[:, ct, :]) for ct in range(2)], rck, [kb])
            fnorm(P, C, [(b[:], kb)], 128, 128.0, 512, [(KT[b2][:, hd, :], gkn_s[:, 0:1], b[:], kb, ("KT", b2))], ["gkn"])
        for sub in range(4):
            for hf in range(2):
                b, kb = bank()
                P.mm(b[:], [(ckn[:, ct, sub * 128:(sub + 1) * 128], ukv[:, ct, hf * 512:(hf + 1) * 512])
                            for ct in range(2)], rck, [kb])
                P.copy("act", Vs[b2][:, sub, hf * 512:(hf + 1) * 512], b[:], [kb], [("Vs", b2)])
        QT_v = S["QT"].rearrange("(h f) t -> f h t", f=192)
        P.dma("pool", ("stq", b2), [(QT_v[0:128, :, t0:t0 + 512], QT[b2][:]), (QT_v[128:192, :, t0:t0 + 512], QR[b2][:])],
              reads=[("QT", b2), ("QR", b2)])
        P.dma("pool", ("stk", b2), [(S["KTh"][h][:, t0:t0 + 512], KT[b2][:, h, :]) for h in range(8)]
              + [(S["KR"][:, t0:t0 + 512], KR[b2][:]), (tok_view(S["Vb"][blk], 0), Vs[b2][:])],
              reads=[("KT", b2), ("KR", b2), ("Vs", b2)])
    P.end()


def emit_mla_attn(P, K, S, G, OT, negb, T):
    P.begin()
    nqb = T // 512
    nkp = T // 128
    Kn = [P.sb("Kn%d" % i, [128, 2, T], BF16) for i in range(2)]
    Kr = P.sb("Kr", [128, 2, T], BF16)
    V = [P.sb("V%d" % i, [128, 2 * nkp, 128], BF16) for i in range(2)]
    Qn = [P.sb("Qn%d" % i, [128, T], BF16) for i in range(2)]
    Qr = [P.sb("Qr%d" % i, [128, T], BF16) for i in range(2)]
    acc = [P.sb("acc%d" % i, [128, 512], F32) for i in range(2)]
    ones32 = P.sb("ones32", [128, 128], F32)
    msk = P.sb("msk", [128, 4, 512], BF16)
    nb = P.sb("nb", [128, 1], F32)
    PT = [P.sb("PT%d" % i, [128, 512], BF16) for i in range(3)]
    rL = P.sb("rL", [128, 512], F32)
    oT = [P.sb("oT%d" % i, [128, 512], BF16) for i in range(2)]
    psT = [P.ps("psT%d" % i, [128, 512], F32) for i in range(3)]
    po = [P.ps("po%d" % i, [128, 512], F32) for i in range(2)]
    pL = [P.ps("pL%d" % i, [128, 512], F32) for i in range(2)]
    P.dma("pool", "msk", [(msk[:], K["cmask"])], writes=["msk"])
    P.add("pool", lambda e: e.memset(ones32[:], 1.0), [], ["ones"])
    P.add("pool", lambda e: e.memset(Kr[64:128, :, :], 0.0), [], ["Krz"])
    for i in range(2):
        P.add("pool", lambda e, i=i: e.memset(Qr[i][64:128, :], 0.0), [], [("Qrz", i)])
    P.dma("sp", "nb", [(nb[:], negb)], writes=["nb"])
    P.dma("sp", "Kr", [(Kr[0:64, 0, :], G["KR"][0:64, :]), (Kr[0:64, 1, :], S["KR"])], writes=["Kr"])
    QT_v = S["QT"].rearrange("(h f) t -> f h t", f=192)
    scale = 192.0 ** -0.5
    LOOK = 2
    units = []
    u = 0
    for hd in range(8):
        for qb in range(nqb):
            kbs = [(0, kb) for kb in range(nkp)] + [(1, kb) for kb in range(4 * (qb + 1))]
            for idx, (half, kb) in enumerate(kbs):
                units.append((hd, qb, half, kb, idx == 0, idx == len(kbs) - 1, u % 2))
            u += 1
    loaded = set()

    def load_head(hd):
        if hd in loaded or hd >= 8:
            return
        loaded.add(hd)
        h2 = hd % 2
        cs_ = slice(hd * 128, (hd + 1) * 128)
        P.dma("sp", ("ldk", h2), [(Kn[h2][:, 0, :], G["KTh"][hd][0:128, :]),
                                  (Kn[h2][:, 1, :], S["KTh"][hd]),
                                  (Qn[h2][:], QT_v[0:128, hd, :]), (Qr[h2][0:64, :], QT_v[128:192, hd, :])],
              writes=[("K", h2)])
        vpairs = []
        for vb in range(nqb):
            k0 = vb * 4
            vpairs.append((V[h2][:, k0:k0 + 4, :], G["Vb"][vb][0:512, cs_].rearrange("(k p) c -> p k c", p=128)))
            vpairs.append((V[h2][:, nkp + k0:nkp + k0 + 4, :], S["Vb"][vb][:, cs_].rearrange("(k p) c -> p k c", p=128)))
        P.dma("sp", ("ldv", h2), vpairs, writes=[("V", h2)])

    def score(n):
        hd, qb, half, kb, first, last, j = units[n]
        h2 = hd % 2
        i3 = n % 3
        qs = slice(qb * 512, (qb + 1) * 512)
        ks = slice(kb * 128, (kb + 1) * 128)
        load_head(hd)
        P.mm(psT[i3][:], [(Kn[h2][:, half, ks], Qn[h2][:, qs]), (Kr[:, half, ks], Qr[h2][:, qs])],
             [("K", h2), "Kr", "Krz", ("Qrz", h2)], [("psT", i3)])

    load_head(0)
    for n in range(min(LOOK, len(units))):
        score(n)
    for n in range(len(units)):
        hd, qb, half, kb, first, last, j = units[n]
        h2 = hd % 2
        i3 = n % 3
        qs = slice(qb * 512, (qb + 1) * 512)
        cs_ = slice(hd * 128, (hd + 1) * 128)
        if half == 0:
            P.actf(PT[i3][:], psT[i3][:], AF.Exp, [("psT", i3), "nb"], [("PT", i3)], scale=scale, bias=nb[:, 0:1])
        else:
            P.actf(PT[i3][:], psT[i3][:], AF.Exp, [("psT", i3)], [("PT", i3)], scale=scale)
            if kb >= 4 * qb:
                P.tt("pool", PT[i3][:], PT[i3][:], msk[:, kb - 4 * qb, :], ALU.mult, [("PT", i3), "msk"], [("PT", i3)])
        if n + LOOK < len(units):
            score(n + LOOK)
        P.mm1(po[j][:], V[h2][:, half * nkp + kb, :], PT[i3][:], first, last, [("PT", i3), ("V", h2)], [("po", j)])
        if first:
            P.copy("dve", acc[j][:], PT[i3][:], [("PT", i3)], [("acc", j)])
        else:
            P.tt("dve", acc[j][:], acc[j][:], PT[i3][:], ALU.add, [("acc", j), ("PT", i3)], [("acc", j)])
        if last:
            P.mm(pL[j][:], [(ones32[:, :], acc[j][:])], [("acc", j), "ones"], [("pL", j)])
            P.add("dve", lambda e, o=rL[:], i=pL[j][:]: e.reciprocal(out=o, in_=i), [("pL", j)], ["rL"])
            P.tt("dve", oT[j][:], po[j][:], rL[:], ALU.mult, [("po", j), "rL"], [("oT", j)])
            P.dma("pool", ("sto", j), [(OT[cs_, qs], oT[j][:])], reads=[("oT", j)])
            if qb == 1:
                load_head(hd + 1)
    P.end()


BIG = ["ffn1_w_gate", "ffn1_w_up", "ffn1_w_down", "ffn2_w_gate", "ffn2_w_up", "ffn2_w_down", "w_out", "mem_w_kv",
       "gla_w_in", "gla_w_alpha", "mla_w_in", "mla_w_uq", "mla_w_ukv"]


def host_layout(inp, T, n_cores):
    f = lambda a: np.ascontiguousarray(np.asarray(a, dtype=np.float32))
    sh = {}
    for k in BIG:
        sh[k] = f(inp[k])
    pk = lambda a: f(np.asarray(a).reshape(a.shape[0], -1, 128).transpose(0, 2, 1))
    for k in ("ffn1_norm", "ffn2_norm", "mix_norm", "mem_norm", "mla_q_norm", "mla_kv_norm",
              "memq_norm", "memk_norm", "mla_qn_norm", "mla_kn_norm"):
        sh[k] = pk(np.asarray(inp[k]))
    for k in ("mla_qr_norm", "mla_kr_norm"):
        g = np.asarray(inp[k], dtype=np.float32)
        sh[k] = f(np.stack([g, np.concatenate([g[:, 32:], g[:, :32]], axis=1)], axis=2))
    ba = np.asarray(inp["gla_b_alpha"], dtype=np.float32)
    sh["gla_b_alpha"] = f(np.broadcast_to(ba[:, None, :], (ba.shape[0], 128, 512)))
    go = np.tile(np.asarray(inp["gla_out_norm"], dtype=np.float32), (1, 4))
    sh["gla_out_norm"] = f(np.broadcast_to(go[:, None, :], (go.shape[0], 128, 1024)))
    j = np.arange(128)
    tri = (j[:, None] <= j[None, :]).astype(np.float32)
    sh["c_ident"] = np.eye(128, dtype=np.float32)
    sh["c_TRI"] = tri
    sh["c_R1"] = f(tri - (j[:, None] <= 63).astype(np.float32))
    sh["c_M3"] = (j[:, None] > j[None, :]).astype(np.float32)
    qq = np.arange(512)
    sh["c_cmask"] = f(np.stack([(jd * 128 + j[:, None] <= qq[None, :]) for jd in range(4)], axis=1).astype(np.float32))
    x = np.asarray(inp["x"], dtype=np.float32)
    mem = np.asarray(inp["mem"], dtype=np.float32)
    inv = (1.0 / (np.float32(10000.0) ** (np.arange(0, 64, 2, dtype=np.float32) / np.float32(64)))).astype(np.float32)
    maps = []
    for c in range(n_cores):
        b, hf = c // 2, c % 2
        m = dict(sh)
        m["x"] = f(x[b, hf * T:(hf + 1) * T])
        m["mem"] = f(mem[b])
        pos = np.arange(hf * T, (hf + 1) * T, dtype=np.float32)
        ang = (pos[None, :] * inv[:, None]).astype(np.float32)
        cs, sn = np.cos(ang).astype(np.float32), np.sin(ang).astype(np.float32)
        m["c_cos"] = f(np.concatenate([cs, cs], axis=0))
        m["c_sin"] = f(np.concatenate([-sn, sn], axis=0))
        m["c_flag"] = np.full((128, 1), float(hf), np.float32)
        m["c_negb"] = np.full((128, 1), 0.0 if hf else -30000.0, np.float32)
        maps.append(m)
    return maps


def build_program(T, depth, maps0, n_cores):
    nc = bass.Bass("TRN2", target_bir_lowering=False)
    I = {}
    for k, v in maps0.items():
        I[k] = nc.dram_tensor(k, list(v.shape), F32, kind="ExternalInput").ap()
    out = nc.dram_tensor("out", [T, D], F32, kind="ExternalOutput").ap()
    NCH = T // 128

    def scr(name, shape, dt):
        return nc.dram_tensor(name, shape, dt)
    X = scr("X", [T, D], F32).ap()
    OT = scr("OT", [1536, T], BF16).ap()
    SG = {k: scr("g_" + k, [512, T], BF16).ap() for k in ("QpT", "KpT", "QppT")}
    SG["Kpp"] = scr("g_Kpp", [T, 512], BF16).ap()
    SG["V"] = scr("g_V", [T, 1024], BF16).ap()
    SG["Rg"] = scr("g_Rg", [T, 1024], BF16).ap()
    SG["DEC"] = scr("g_DEC", [128, 4 * NCH], F32).ap()
    Sbn = scr("g_Sb", [128, 1024], F32)
    Sgt = scr("g_Sg", [256, 1024], F32)
    nblk = T // 512
    SMt = {"KTh": [scr("m_KT%d" % h, [128, T], BF16) for h in range(8)], "KR": scr("m_KR", [64, T], BF16),
           "Vb": [scr("m_V%d" % b, [512, 1024], BF16) for b in range(nblk)]}
    GMt = {"KTh": [scr("m_KTg%d" % h, [256, T], BF16) for h in range(8)], "KR": scr("m_KRg", [128, T], BF16),
           "Vb": [scr("m_Vg%d" % b, [1024, 1024], BF16) for b in range(nblk)]}
    SM = {"QT": scr("m_QT", [1536, T], BF16).ap(), "KTh": [t.ap() for t in SMt["KTh"]], "KR": SMt["KR"].ap(),
          "Vb": [t.ap() for t in SMt["Vb"]]}
    GM = {"KTh": [t.ap() for t in GMt["KTh"]], "KR": GMt["KR"].ap(), "Vb": [t.ap() for t in GMt["Vb"]]}
    groups = [[2 * i, 2 * i + 1] for i in range(n_cores // 2)]
    K = {"ident": I["c_ident"], "TRI": I["c_TRI"], "R1": I["c_R1"], "M3": I["c_M3"], "cmask": I["c_cmask"]}
    with ExitStack() as es:
        P = Prog(nc, es)
        for i in range(depth):
            j = i // 2
            src = I["x"] if i == 0 else X
            emit_ffn(P, K, src, X, I["ffn1_w_gate"][i], I["ffn1_w_up"][i], I["ffn1_w_down"][i], I["ffn1_norm"][i], T)
            if i % 2 == 0:
                emit_gla_proj(P, K, X, I["gla_w_in"][j], I["mix_norm"][i], I["gla_w_alpha"][j], I["gla_b_alpha"][j],
                              I["gla_out_norm"][j], SG, T)
                win_v = I["gla_w_in"][j].rearrange("(kt p) f -> p kt f", p=128)
                emit_mem(P, K, X, OT, win_v, 3088, I["mix_norm"][i], I["mem"], I["mem_norm"][i], I["mem_w_kv"][i],
                         I["memq_norm"][i], I["memk_norm"][i], T)
                emit_gla_scan(P, K, SG, OT, T, False, Sbounce=Sbn.ap())
                P.begin()
                _cc(P, "ccS", Sbn.ap().opt(), Sgt.ap().opt(), groups, [], [])
                P.end()
                emit_gla_scan(P, K, SG, OT, T, True, Sgath=Sgt.ap(), flag=I["c_flag"])
            else:
                emit_mla_proj(P, K, X, I["mla_w_in"][j], I["mix_norm"][i], I["mla_q_norm"][j], I["mla_kv_norm"][j],
                              I["mla_w_uq"][j], I["mla_w_ukv"][j], I["mla_qn_norm"][j], I["mla_qr_norm"][j],
                              I["mla_kn_norm"][j], I["mla_kr_norm"][j], I["c_cos"], I["c_sin"], SM, T)
                win_v = I["mla_w_in"][j].rearrange("(kt p) f -> p kt f", p=128)
                emit_mem(P, K, X, OT, win_v, 704, I["mix_norm"][i], I["mem"], I["mem_norm"][i], I["mem_w_kv"][i],
                         I["memq_norm"][i], I["memk_norm"][i], T)
                P.begin()
                pieces = [(SMt["KR"], GMt["KR"])] + list(zip(SMt["KTh"], GMt["KTh"])) + list(zip(SMt["Vb"], GMt["Vb"]))
                for ci, (a, g) in enumerate(pieces):
                    _cc(P, ("ccM", ci), a.ap().opt(), g.ap().opt(), groups, [], [])
                P.end()
                emit_mla_attn(P, K, SM, GM, OT, I["c_negb"], T)
            emit_outproj(P, K, X, OT, I["w_out"][i], T)
            dst = out if i == depth - 1 else X
            emit_ffn(P, K, X, dst, I["ffn2_w_gate"][i], I["ffn2_w_up"][i], I["ffn2_w_down"][i], I["ffn2_norm"][i], T)
        nops = P.n_ops
    return nc, nops


def kernel(**inputs):
    n_cores = 8
    B, S, _ = inputs["x"].shape
    T = S * B // n_cores
    maps = host_layout(inputs, T, n_cores)
    nc, _ = build_program(T, 4, maps[0], n_cores)
    res = run_bass_kernel_spmd(nc, maps, core_ids=list(range(n_cores)))
    y = np.empty((B, S, D), np.float32)
    for c in range(n_cores):
        y[c // 2, (c % 2) * T:(c % 2 + 1) * T] = res.results[c]["out"]
    return y
```

```python
import numpy as np
import concourse.bass as bass
import concourse.mybir as mybir
from concourse.bass_utils import run_bass_kernel_spmd
from contextlib import ExitStack

F32 = mybir.dt.float32
BF16 = mybir.dt.bfloat16
AF = mybir.ActivationFunctionType
ALU = mybir.AluOpType
AX = mybir.AxisListType

D = 1024
DFF = 2816
NF = DFF // 128
EPS = 1e-6


class Op:
    __slots__ = ("eng", "fn", "is_dma", "clock", "pos", "waits", "known", "inc", "count", "sem", "ndma")


class Prog:
    ENGS = ("pe", "act", "dve", "pool", "sp")

    def __init__(self, nc, es):
        self.nc = nc
        self.es = es
        self.sem = {e: es.enter_context(nc.semaphore("s_" + e)) for e in self.ENGS}
        self.cnt = {e: 0 for e in self.ENGS}
        self.dma_sem = {}
        self.dma_cnt = {}
        self.sem_pool = {}
        self.sem_used = {}
        self.n_ops = 0
        self._reset()

    def _reset(self):
        self.streams = {e: [] for e in self.ENGS}
        self.last_writer = {}
        self.readers = {}
        self.known = {e: {} for e in self.ENGS}
        self.dma_pos = {}
        self.blk_dma = {}

    def _record(self, op, reads, writes):
        eng = op.eng
        raw = set()
        deps = set()
        for k in reads:
            w = self.last_writer.get(k)
            if w is not None:
                deps.add(w)
                raw.add(w)
        for k in writes:
            w = self.last_writer.get(k)
            if w is not None:
                deps.add(w)
            for r in self.readers.get(k, {}).values():
                deps.add(r)
        kn = self.known[eng]
        op.waits = []
        for d in sorted(deps, key=lambda d: -d.pos):
            if d.clock == eng and eng == "pe":
                continue
            if kn.get(d.clock, 0) >= d.pos:
                continue
            op.waits.append(d)
            for c, p in d.known.items():
                if kn.get(c, 0) < p:
                    kn[c] = p
            if kn.get(d.clock, 0) < d.pos:
                kn[d.clock] = d.pos
        op.known = dict(kn)
        self.streams[eng].append(op)
        for k in reads:
            self.readers.setdefault(k, {})[op.clock] = op
        for k in writes:
            self.last_writer[k] = op
            self.readers[k] = {}
        self.n_ops += 1

    def add(self, eng, fn, reads=(), writes=()):
        op = Op()
        op.eng = eng
        op.fn = fn
        op.is_dma = False
        op.clock = eng
        op.pos = len(self.streams[eng]) + 1
        op.inc = False
        op.count = None
        op.sem = self.sem[eng]
        self._record(op, reads, writes)
        return op

    def dma(self, eng, key, pairs, reads=(), writes=(), **kw):
        self._dma_key(key, "sw" if eng == "pool" else "hw")
        op = Op()
        op.eng = eng
        op.is_dma = True
        op.clock = ("dma", key)
        self.dma_pos[key] = self.dma_pos.get(key, 0) + 1
        op.pos = self.dma_pos[key]
        op.inc = True
        op.ndma = len(pairs)
        self.dma_cnt[key] += 16 * len(pairs)
        op.count = self.dma_cnt[key]
        op.sem = self.dma_sem[key]
        sem = op.sem

        def fn(e, pairs=pairs, sem=sem, kw=kw):
            for (o, i) in pairs:
                e.dma_start(out=o, in_=i, **kw).then_inc(sem, 16)
        op.fn = fn
        self._record(op, reads, writes)
        self.blk_dma[key] = op
        return op

    def _dma_key(self, key, kind):
        if key not in self.dma_sem:
            pool = self.sem_pool.setdefault(kind, [])
            used = self.sem_used.get(kind, 0)
            if used == len(pool):
                pool.append([self.es.enter_context(self.nc.semaphore("d%s_%d" % (kind, len(pool)))), 0])
            ent = pool[used]
            self.sem_used[kind] = used + 1
            self.dma_sem[key] = ent[0]
            self.dma_cnt[key] = ent[1]
            self.key_ent = getattr(self, "key_ent", {})
            self.key_ent[key] = ent

    def emit(self):
        nc = self.nc
        for key, op in self.blk_dma.items():
            f = Op()
            f.eng = op.eng
            f.fn = None
            f.is_dma = False
            f.clock = op.eng
            f.pos = len(self.streams[op.eng]) + 1
            f.inc = False
            f.count = None
            f.sem = None
            f.waits = [op]
            f.known = {}
            self.streams[op.eng].append(f)
        for e in self.ENGS:
            for op in self.streams[e]:
                for t in op.waits:
                    t.inc = True
        for e in self.ENGS:
            c = self.cnt[e]
            for op in self.streams[e]:
                if op.is_dma or op.fn is None:
                    continue
                if op.inc:
                    c += 1
                    op.count = c
            self.cnt[e] = c
        reg = {"pe": "tensor", "act": "scalar", "dve": "vector", "pool": "gpsimd", "sp": "sync"}
        with nc.Block() as block:
            for e in self.ENGS:
                ops = self.streams[e]
                mysem = self.sem[e]

                def body(eng, ops=ops, mysem=mysem):
                    for op in ops:
                        for t in op.waits:
                            eng.wait_ge(t.sem, t.count)
                        if op.fn is None:
                            continue
                        if op.is_dma:
                            op.fn(eng)
                        else:
                            ins = op.fn(eng)
                            if op.inc:
                                ins.then_inc(mysem, 1)
                getattr(block, reg[e])(body)
        for key, ent in getattr(self, "key_ent", {}).items():
            ent[1] = self.dma_cnt[key]
        self.key_ent = {}
        self.dma_sem = {}
        self.dma_cnt = {}
        self.sem_used = {}
        self._reset()

    def begin(self):
        self.pes = ExitStack()
        self.nph = getattr(self, "nph", 0) + 1

    def end(self):
        self.emit()
        self.pes.close()

    def sb(self, name, shape, dtype):
        return self.pes.enter_context(self.nc.sbuf_tensor("sb%d_%s" % (self.nph, name), shape, dtype))

    def ps(self, name, shape, dtype):
        return self.pes.enter_context(self.nc.psum_tensor("ps%d_%s" % (self.nph, name), shape, dtype))

    def mm(self, out, pairs, reads, writes, **kw):
        n = len(pairs)

        def fn(e, out=out, pairs=pairs, n=n, kw=kw):
            ins = None
            for i, (l, r) in enumerate(pairs):
                ins = e.matmul(out, lhsT=l, rhs=r, start=(i == 0), stop=(i == n - 1), **kw)
            return ins
        return self.add("pe", fn, reads, writes)

    def mm1(self, out, lhsT, rhs, first, last, reads, writes):
        return self.add("pe", lambda e, o=out, l=lhsT, r=rhs, f=first, la=last:
                        e.matmul(o, lhsT=l, rhs=r, start=f, stop=la), reads, writes)

    def tr(self, out, in_, ident, reads, writes):
        return self.add("pe", lambda e, o=out, i=in_, d=ident: e.transpose(o, i, d), reads, writes)

    def actf(self, out, in_, func, reads, writes, eng="act", **kw):
        return self.add(eng, lambda e, o=out, i=in_, f=func, kw=kw: e.activation(out=o, in_=i, func=f, **kw),
                        reads, writes)

    def tt(self, eng, out, in0, in1, op, reads, writes):
        return self.add(eng, lambda e, o=out, a=in0, b=in1, op=op: e.tensor_tensor(out=o, in0=a, in1=b, op=op),
                        reads, writes)

    def ts(self, eng, out, in0, s1, s2, op0, op1, reads, writes):
        if op1 is None:
            return self.add(eng, lambda e, o=out, a=in0, s1=s1, op0=op0:
                            e.tensor_scalar(out=o, in0=a, scalar1=s1, scalar2=None, op0=op0), reads, writes)
        return self.add(eng, lambda e, o=out, a=in0, s1=s1, s2=s2, op0=op0, op1=op1:
                        e.tensor_scalar(out=o, in0=a, scalar1=s1, scalar2=s2, op0=op0, op1=op1), reads, writes)

    def stt(self, out, in0, scalar, in1, op0, op1, reads, writes):
        return self.add("dve", lambda e, o=out, a=in0, s=scalar, b=in1, op0=op0, op1=op1:
                        e.scalar_tensor_tensor(out=o, in0=a, scalar=s, in1=b, op0=op0, op1=op1), reads, writes)

    def copy(self, eng, out, in_, reads, writes):
        if eng == "act":
            return self.add(eng, lambda e, o=out, i=in_: e.copy(out=o, in_=i), reads, writes)
        return self.add(eng, lambda e, o=out, i=in_: e.tensor_copy(out=o, in_=i), reads, writes)


class Ctx:
    pass


def load_norm(P, C, src, blk, tag):
    for sub in range(4):
        t0 = blk * 512 + sub * 128
        xs = C.xs[sub % 2]
        kxs = ("xs", sub % 2)
        P.dma("sp", kxs, [(xs[:], src[t0:t0 + 128, :])], reads=[("dram", tag, blk)], writes=[kxs])
        P.add("act", lambda e, o=C.junk[:], i=xs[:], a=C.ss[:, sub:sub + 1]:
              e.activation(out=o, in_=i, func=AF.Square, accum_out=a), [kxs], ["junk", ("ss", sub)])
        P.ts("pool", C.sq[:, sub:sub + 1], C.ss[:, sub:sub + 1], float(D) * EPS, None, ALU.add, None,
             [("ss", sub)], [("sq", sub)])
        P.tt("pool", C.rstd[:, sub:sub + 1], C.sq[:, sub:sub + 1], C.mhalf[:, 0:1], ALU.pow,
             [("sq", sub), "mhalf"], [("rstd", sub)])
        P.ts("dve", C.xn[:, sub, :], xs[:], C.rstd[:, sub:sub + 1], float(D) ** 0.5, ALU.mult, ALU.mult,
             [kxs, ("rstd", sub)], [("xn", sub)])


def transpose_hT(P, C, gvec):
    for kt in range(8):
        pt = C.pt[kt % 2][:, 0:512]
        kpt = ("pt", kt % 2)
        for sub in range(4):
            P.tr(pt[:, sub * 128:(sub + 1) * 128], C.xn[:, sub, kt * 128:(kt + 1) * 128], C.ident[:],
                 [("xn", sub), "ident"], [kpt])
        P.ts("dve", C.hT[:, kt, :], pt, gvec[:, kt:kt + 1], None, ALU.mult, None, [kpt, "gvec"], [("hT", kt)])


def _cc(P, key, ins, outs, groups, reads, writes):
    P._dma_key(key, "cc")
    op = Op()
    op.eng = "pool"
    op.is_dma = True
    op.clock = ("dma", key)
    P.dma_pos[key] = P.dma_pos.get(key, 0) + 1
    op.pos = P.dma_pos[key]
    op.inc = True
    P.dma_cnt[key] += 1
    op.count = P.dma_cnt[key]
    op.sem = P.dma_sem[key]
    sem = op.sem

    def fn(e):
        e.collective_compute("AllGather", ALU.bypass, replica_groups=groups, ins=[ins], outs=[outs]).then_inc(sem)
    op.fn = fn
    P._record(op, reads, writes)
    P.blk_dma[key] = op


def alloc_xin(P, C, K, gnorm):
    C.xs = [P.sb("xs%d" % i, [128, D], F32) for i in range(2)]
    C.junk = P.sb("junk", [128, D], BF16)
    C.xn = P.sb("xn", [128, 4, D], BF16)
    C.hT = P.sb("hT", [128, 8, 512], BF16)
    C.gvec = P.sb("gvec", [128, 8], F32)
    C.ss = P.sb("ss", [128, 4], F32)
    C.sq = P.sb("sq", [128, 4], F32)
    C.rstd = P.sb("rstd", [128, 4], F32)
    C.mhalf = P.sb("mhalf", [128, 4], F32)
    C.ident = P.sb("ident", [128, 128], BF16)
    C.pt = [P.ps("pt%d" % i, [128, 1024], BF16) for i in range(2)]
    P.dma("pool", "ident", [(C.ident[:], K["ident"])], writes=["ident"])
    P.add("pool", lambda e: e.memset(C.mhalf[:], -0.5), [], ["mhalf"])
    P.dma("sp", "gvec", [(C.gvec[:], gnorm)], writes=["gvec"])


def load_w(P, key, dst, src_v, n0, n1, step=512):
    nk = src_v.shape[1]
    keys = []
    for lo in range(n0, n1, step):
        hi = min(n1, lo + step)
        k = (key, (lo - n0) // step)
        P.dma("pool", k, [(dst[:, kt, lo - n0:hi - n0], src_v[:, kt, lo:hi]) for kt in range(nk)], writes=[k])
        keys.append(k)
    return keys


def tok_view(ap, t0, nsub=4):
    return ap[t0:t0 + nsub * 128, :].rearrange("(s p) c -> p s c", p=128)


def emit_ffn(P, K, src, dst, wg, wu, wd, gnorm, T, tag_src="x", tag_dst="x"):
    P.begin()
    C = Ctx()
    nblk = T // 512
    alloc_xin(P, C, K, gnorm)
    C.wg = P.sb("wg", [128, 8, DFF], BF16)
    C.wu = P.sb("wu", [128, 8, DFF], BF16)
    C.wd = P.sb("wd", [128, NF, D], BF16)
    C.actT = P.sb("actT", [128, NF, 512], BF16)
    C.xres = P.sb("xres", [128, 4, D], F32)
    C.sg = [P.sb("sg%d" % i, [128, 512], F32) for i in range(2)]
    C.pg = [P.ps("pg%d" % i, [128, 512], F32) for i in range(2)]
    C.pu = [P.ps("pu%d" % i, [128, 512], F32) for i in range(2)]
    C.pd = [P.ps("pd%d" % i, [128, 512], F32) for i in range(2)]
    wg_v = wg.rearrange("(kt p) f -> p kt f", p=128)
    wu_v = wu.rearrange("(kt p) f -> p kt f", p=128)
    wd_v = wd.rearrange("(ft p) d -> p ft d", p=128)
    nch = (DFF + 511) // 512
    for c in range(nch):
        lo, hi = c * 512, min(DFF, c * 512 + 512)
        P.dma("pool", ("wg", c), [(C.wg[:, kt, lo:hi], wg_v[:, kt, lo:hi]) for kt in range(8)], writes=[("wg", c)])
        P.dma("pool", ("wu", c), [(C.wu[:, kt, lo:hi], wu_v[:, kt, lo:hi]) for kt in range(8)], writes=[("wu", c)])
    for c in range(6):
        fts = list(range(c * 4, min(NF, c * 4 + 4)))
        P.dma("pool", ("wd", c), [(C.wd[:, ft, :], wd_v[:, ft, :]) for ft in fts], writes=[("wd", c)])
    load_norm(P, C, src, 0, tag_src)
    transpose_hT(P, C, C.gvec)
    for blk in range(nblk):
        for f in range(NF):
            pg = C.pg[f % 2]
            pu = C.pu[f % 2]
            P.mm(pg[:], [(C.wg[:, kt, f * 128:(f + 1) * 128], C.hT[:, kt, :]) for kt in range(8)],
                 [("wg", f // 4)] + [("hT", kt) for kt in range(8)], [("pg", f % 2)])
            P.mm(pu[:], [(C.wu[:, kt, f * 128:(f + 1) * 128], C.hT[:, kt, :]) for kt in range(8)],
                 [("wu", f // 4)] + [("hT", kt) for kt in range(8)], [("pu", f % 2)])
            sg = C.sg[f % 2]
            P.actf(sg[:], pg[:], AF.Silu, [("pg", f % 2)], [("sg", f % 2)])
            P.tt("dve", C.actT[:, f, :], sg[:], pu[:], ALU.mult, [("sg", f % 2), ("pu", f % 2)], [("actT", f)])
            if f == 6 and blk + 1 < nblk:
                load_norm(P, C, src, blk + 1, tag_src)
        t0 = blk * 512
        P.dma("sp", "xres", [(C.xres[:], tok_view(src, t0))], reads=[("dram", tag_src, blk)], writes=["xres"])
        if blk + 1 < nblk:
            transpose_hT(P, C, C.gvec)
        for sub in range(4):
            for dh in range(2):
                pd = C.pd[(sub * 2 + dh) % 2]
                kpd = ("pd", (sub * 2 + dh) % 2)
                P.mm(pd[:], [(C.actT[:, f, sub * 128:(sub + 1) * 128], C.wd[:, f, dh * 512:(dh + 1) * 512])
                             for f in range(NF)],
                     [("wd", c) for c in range(6)] + [("actT", f) for f in range(NF)], [kpd])
                xr = C.xres[:, sub, dh * 512:(dh + 1) * 512]
                P.stt(xr, pd[:], 0.5, xr, ALU.mult, ALU.add, [kpd, "xres"], ["xres"])
        P.dma("pool", "xst", [(tok_view(dst, t0), C.xres[:])], reads=["xres"], writes=[("dram", tag_dst, blk)])
    P.end()


def emit_outproj(P, K, X, OT, wout, T):
    P.begin()
    nblk = T // 512
    wo = P.sb("wo", [128, 12, D], BF16)
    ot = [P.sb("ot%d" % i, [128, 12, 512], BF16) for i in range(2)]
    xres = P.sb("xres", [128, 4, D], F32)
    pd = [P.ps("pd%d" % i, [128, 512], F32) for i in range(4)]
    wo_v = wout.rearrange("(ft p) d -> p ft d", p=128)
    for c in range(3):
        P.dma("pool", ("wo", c), [(wo[:, ft, :], wo_v[:, ft, :]) for ft in range(c * 4, c * 4 + 4)], writes=[("wo", c)])
    OT_v = OT.rearrange("(ft p) t -> p ft t", p=128)
    for blk in range(nblk):
        t0 = blk * 512
        o = ot[blk % 2]
        ko = ("ot", blk % 2)
        P.dma("sp", ko, [(o[:], OT_v[:, :, t0:t0 + 512])], writes=[ko])
        P.dma("sp", "xres", [(xres[:], tok_view(X, t0))], writes=["xres"])
        for sub in range(4):
            for dh in range(2):
                i = (sub * 2 + dh) % 4
                P.mm(pd[i][:], [(o[:, ft, sub * 128:(sub + 1) * 128], wo[:, ft, dh * 512:(dh + 1) * 512])
                                for ft in range(12)], [ko] + [("wo", c) for c in range(3)], [("pd", i)])
                xr = xres[:, sub, dh * 512:(dh + 1) * 512]
                P.tt("dve", xr, pd[i][:], xr, ALU.add, [("pd", i), "xres"], ["xres"])
        P.dma("pool", "xst", [(tok_view(X, t0), xres[:])], reads=["xres"])
    P.end()


def alloc_fnorm(P, C):
    C.sqb = [P.sb("sqb%d" % i, [128, 512], BF16) for i in range(3)]
    C.lnb = P.sb("lnb", [128, 512], F32)
    C.rsb = P.sb("rsb", [128, 512], F32)
    C.ones = P.sb("ones", [128, 128], BF16)
    C.epsT = P.sb("epsT", [128, 1], F32)
    C.oneT = P.sb("oneT", [128, 1], F32)
    C.pss = P.ps("pss", [128, 512], F32)
    P.add("pool", lambda e: e.memset(C.ones[:], 1.0), [], ["ones"])
    P.add("pool", lambda e: e.memset(C.epsT[:], EPS), [], ["epsT"])
    P.add("pool", lambda e: e.memset(C.oneT[:], 1.0), [], ["oneT"])


def fnorm(P, C, srcs, np_, nfeat, ncol, outs, extra_reads=()):
    n = len(srcs)
    for i, (src, k) in enumerate(srcs):
        P.actf(C.sqb[i][0:np_, 0:ncol], src, AF.Square, [k], [("sqb", i)])
    P.mm(C.pss[0:np_, 0:ncol], [(C.ones[0:np_, 0:np_], C.sqb[i][0:np_, 0:ncol]) for i in range(n)],
         [("sqb", i) for i in range(n)] + ["ones"], ["pss"])
    P.actf(C.lnb[0:np_, 0:ncol], C.pss[0:np_, 0:ncol], AF.Ln, ["pss", "epsT"], ["lnb"],
           scale=1.0 / nfeat, bias=C.epsT[0:np_, 0:1])
    P.actf(C.rsb[0:np_, 0:ncol], C.lnb[0:np_, 0:ncol], AF.Exp, ["lnb"], ["rsb"], scale=-0.5)
    for (o, g, s, sk, ok) in outs:
        P.stt(o, s, g, C.rsb[0:np_, 0:ncol], ALU.mult, ALU.mult, [sk, "rsb"] + list(extra_reads), [ok])


def emit_mem(P, K, X, OT, wq_v, ncol0, gmix, mem, gmem, wkv, gq, gk, T, pre=None):
    P.begin()
    C = Ctx()
    nblk = T // 512
    alloc_xin(P, C, K, gmix)
    alloc_fnorm(P, C)
    wq = P.sb("wq", [128, 8, 512], BF16)
    wk = P.sb("wkv", [128, 8, 1024], BF16)
    gm = P.sb("gm", [128, 8], F32)
    gqs = P.sb("gqs", [128, 1], F32)
    gks = P.sb("gks", [128, 1], F32)
    memT = P.sb("memT", [128, 8, 256], BF16)
    KmT = P.sb("KmT", [128, 4, 256], BF16)
    Vm = P.sb("Vm", [128, 2, 512], BF16)
    qmn = [P.sb("qmn%d" % i, [128, 512], BF16) for i in range(2)]
    PT = [P.sb("PT%d" % i, [128, 2, 512], BF16) for i in range(2)]
    rL = P.sb("rL", [128, 512], F32)
    omT = [P.sb("omT%d" % i, [128, 4, 512], BF16) for i in range(2)]
    pq = [P.ps("pq0", [128, 512], F32)] * 2
    psT = [P.ps("psT%d" % i, [128, 512], F32) for i in range(2)]
    po = P.ps("po", [128, 512], F32)
    pL = P.ps("pL", [128, 512], F32)
    kq = load_w(P, "wq", wq, wq_v, ncol0, ncol0 + 512)
    kk = load_w(P, "wkv", wk, wkv.rearrange("(kt p) f -> p kt f", p=128), 0, 1024)
    if pre is not None:
        pre()
    P.dma("sp", "gm", [(gm[:], gmem)], writes=["gm"])
    P.dma("sp", "gqs", [(gqs[:], gq)], writes=["gqs"])
    P.dma("sp", "gks", [(gks[:], gk)], writes=["gks"])
    for sub in range(2):
        xs = C.xs[sub]
        kxs = ("xs", sub)
        P.dma("sp", kxs, [(xs[:], mem[sub * 128:(sub + 1) * 128, :])], writes=[kxs])
        P.add("act", lambda e, o=C.junk[:], i=xs[:], a=C.ss[:, sub:sub + 1]:
              e.activation(out=o, in_=i, func=AF.Square, accum_out=a), [kxs], ["junk", ("ss", sub)])
        P.ts("pool", C.sq[:, sub:sub + 1], C.ss[:, sub:sub + 1], float(D) * EPS, None, ALU.add, None,
             [("ss", sub)], [("sq", sub)])
        P.tt("pool", C.rstd[:, sub:sub + 1], C.sq[:, sub:sub + 1], C.mhalf[:, 0:1], ALU.pow,
             [("sq", sub), "mhalf"], [("rstd", sub)])
        P.ts("dve", C.xn[:, sub, :], xs[:], C.rstd[:, sub:sub + 1], float(D) ** 0.5, ALU.mult, ALU.mult,
             [kxs, ("rstd", sub)], [("xn", sub)])
    for kt in range(8):
        pt = C.pt[kt % 2][:, 0:256]
        kpt = ("pt", kt % 2)
        for sub in range(2):
            P.tr(pt[:, sub * 128:(sub + 1) * 128], C.xn[:, sub, kt * 128:(kt + 1) * 128], C.ident[:],
                 [("xn", sub), "ident"], [kpt])
        P.ts("dve", memT[:, kt, :], pt, gm[:, kt:kt + 1], None, ALU.mult, None, [kpt, "gm"], [("memT", kt)])
    rmem = [("memT", kt) for kt in range(8)]
    for hd in range(4):
        p = pq[hd % 2]
        kp = ("pq", 0)
        P.mm(p[:, 0:256], [(wk[:, kt, hd * 128:(hd + 1) * 128], memT[:, kt, :]) for kt in range(8)], rmem + kk, [kp])
        fnorm(P, C, [(p[:, 0:256], kp)], 128, 128.0, 256, [(KmT[:, hd, :], gks[:, 0:1], p[:, 0:256], kp, ("KmT", hd))],
              ["gks"])
    for mt in range(2):
        p = pq[mt % 2]
        kp = ("pq", 0)
        P.mm(p[:], [(memT[:, kt, mt * 128:(mt + 1) * 128], wk[:, kt, 512:1024]) for kt in range(8)], rmem + kk, [kp])
        P.copy("act", Vm[:, mt, :], p[:], [kp], [("Vm", mt)])
    OT_v = OT[1024:1536, :].rearrange("(h p) t -> p h t", p=128)
    scale = 128.0 ** -0.5
    load_norm(P, C, X, 0, "x")
    for blk in range(nblk):
        transpose_hT(P, C, C.gvec)
        if blk + 1 < nblk:
            load_norm(P, C, X, blk + 1, "x")
        om = omT[blk % 2]
        for hd in range(4):
            p = pq[hd % 2]
            kp = ("pq", 0)
            q = qmn[hd % 2]
            kqn = ("qmn", hd % 2)
            P.mm(p[:], [(wq[:, kt, hd * 128:(hd + 1) * 128], C.hT[:, kt, :]) for kt in range(8)],
                 [("hT", kt) for kt in range(8)] + kq, [kp])
            fnorm(P, C, [(p[:], kp)], 128, 128.0, 512, [(q[:], gqs[:, 0:1], p[:], kp, kqn)], ["gqs"])
            pt_ = PT[hd % 2]
            for mt in range(2):
                P.mm(psT[mt][:], [(KmT[:, hd, mt * 128:(mt + 1) * 128], q[:])], [("KmT", hd), kqn], [("psT", mt)])
                P.actf(pt_[:, mt, :], psT[mt][:], AF.Exp, [("psT", mt)], [("PT", hd % 2, mt)], scale=scale)
            rpt = [("PT", hd % 2, mt) for mt in range(2)]
            P.mm(po[:], [(Vm[:, mt, hd * 128:(hd + 1) * 128], pt_[:, mt, :]) for mt in range(2)],
                 rpt + [("Vm", 0), ("Vm", 1)], ["po"])
            P.mm(pL[:], [(C.ones[:, :], pt_[:, mt, :]) for mt in range(2)], rpt + ["ones"], ["pL"])
            P.add("dve", lambda e, o=rL[:], i=pL[:]: e.reciprocal(out=o, in_=i), ["pL"], ["rL"])
            P.tt("dve", om[:, hd, :], po[:], rL[:], ALU.mult, ["po", "rL"], [("om", blk % 2)])
        P.dma("pool", ("omst", blk % 2), [(OT_v[:, :, blk * 512:(blk + 1) * 512], om[:])], reads=[("om", blk % 2)])
    P.end()


GQ, GK_, GV, GA, GR = 0, 512, 1024, 2048, 2064


def emit_gla_proj(P, K, X, win, gmix, walpha, balpha_b, gout_b, S, T):
    P.begin()
    C = Ctx()
    nblk = T // 512
    NCH = T // 128
    alloc_xin(P, C, K, gmix)
    w = P.sb("w", [128, 8, 3088], BF16)
    wal = P.sb("wal", [16, 512], BF16)
    bal = P.sb("bal", [128, 512], F32)
    gob = P.sb("gob", [128, 1024], F32)
    R1 = P.sb("R1", [128, 128], F32)
    TRI = P.sb("TRI", [128, 128], F32)
    M3 = P.sb("M3", [128, 128], F32)
    oneT = P.sb("oneT", [128, 1], F32)
    qT = P.sb("qT", [128, 4, 512], F32)
    kT = P.sb("kT", [128, 4, 512], F32)
    ktok = P.sb("ktok", [128, 4, 512], F32)
    vsb = P.sb("vsb", [128, 4, 1024], BF16)
    alT = P.sb("alT", [16, 512], BF16)
    zb = P.sb("zb", [128, 512], F32)
    la = P.sb("la", [128, 4, 512], F32)
    E = [P.sb("E%d" % i, [128, 512], F32) for i in range(4)]
    QpT = P.sb("QpT", [128, 4, 512], BF16)
    KpT = P.sb("KpT", [128, 4, 512], BF16)
    QppT = P.sb("QppT", [128, 4, 512], BF16)
    Kpp = P.sb("Kpp", [128, 4, 512], BF16)
    DEC = P.sb("DEC", [128, 4, NCH], F32)
    Rg = P.sb("Rg", [128, 4, 1024], BF16)
    sil = P.sb("sil", [128, 512], F32)
    bk = [P.ps("bk%d" % i, [128, 512], F32) for i in range(6)]
    win_v = win.rearrange("(kt p) f -> p kt f", p=128)
    kw = load_w(P, "w", w, win_v, 0, 3088)
    P.dma("pool", "wal", [(wal[:], walpha)], writes=["wal"])
    P.dma("sp", "bal", [(bal[:], balpha_b)], writes=["bal"])
    P.dma("sp", "gob", [(gob[:], gout_b)], writes=["gob"])
    P.dma("sp", "R1", [(R1[:], K["R1"])], writes=["R1"])
    P.dma("sp", "TRI", [(TRI[:], K["TRI"])], writes=["TRI"])
    P.dma("sp", "M3", [(M3[:], K["M3"])], writes=["M3"])
    P.add("pool", lambda e: e.memset(oneT[:], 1.0), [], ["oneT"])
    rh = [("hT", kt) for kt in range(8)]
    nb = [0]

    def bank():
        i = nb[0] % 6
        nb[0] += 1
        return bk[i], ("bk", i)

    load_norm(P, C, X, 0, "x")
    for blk in range(nblk):
        t0 = blk * 512
        transpose_hT(P, C, C.gvec)
        if blk + 1 < nblk:
            load_norm(P, C, X, blk + 1, "x")
        for hd in range(4):
            b, kb = bank()
            P.mm(b[:], [(w[:, kt, GQ + hd * 128:GQ + (hd + 1) * 128], C.hT[:, kt, :]) for kt in range(8)], rh + kw, [kb])
            P.actf(qT[:, hd, :], b[:], AF.Identity, [kb], [("qT", hd)], scale=128.0 ** -0.5)
            b, kb = bank()
            P.mm(b[:], [(w[:, kt, GK_ + hd * 128:GK_ + (hd + 1) * 128], C.hT[:, kt, :]) for kt in range(8)], rh + kw, [kb])
            P.copy("dve", kT[:, hd, :], b[:], [kb], [("kT", hd)])
        b, kb = bank()
        P.mm(b[0:16, :], [(w[:, kt, GA:GA + 16], C.hT[:, kt, :]) for kt in range(8)], rh + kw, [kb])
        P.copy("dve", alT[:], b[0:16, :], [kb], ["alT"])
        for sub in range(4):
            ts_ = slice(sub * 128, (sub + 1) * 128)
            b, kb = bank()
            P.mm(b[:], [(C.hT[:, kt, ts_], w[:, kt, GK_:GK_ + 512]) for kt in range(8)], rh + kw, [kb])
            P.copy("act", ktok[:, sub, :], b[:], [kb], [("ktok", sub)])
            for hf in range(2):
                b, kb = bank()
                P.mm(b[:], [(C.hT[:, kt, ts_], w[:, kt, GV + hf * 512:GV + (hf + 1) * 512]) for kt in range(8)],
                     rh + kw, [kb])
                P.copy("dve", vsb[:, sub, hf * 512:(hf + 1) * 512], b[:], [kb], [("vsb", sub)])
            b, kb = bank()
            P.mm(b[:], [(alT[0:16, ts_], wal[0:16, :])], ["alT", "wal"], [kb])
            P.tt("dve", zb[:], b[:], bal[:], ALU.add, [kb, "bal"], ["zb"])
            P.actf(zb[:], zb[:], AF.Exp, ["zb"], ["zb"], scale=-1.0)
            P.actf(zb[:], zb[:], AF.Ln, ["zb", "oneT"], ["zb"], bias=oneT[:, 0:1])
            P.ts("dve", la[:, sub, :], zb[:], -1.0 / 16.0, None, ALU.mult, None, ["zb"], [("la", sub)])
            bA1, kA1 = bank()
            bA2, kA2 = bank()
            bA3, kA3 = bank()
            for hd in range(4):
                hs = slice(hd * 128, (hd + 1) * 128)
                P.mm(bA1[:, hs], [(la[:, sub, hs], R1[:, :])], [("la", sub), "R1"], [kA1])
                P.mm(bA2[:, hs], [(la[:, sub, hs], TRI[:, :])], [("la", sub), "TRI"], [kA2])
            P.mm(bA3[:], [(M3[:, :], la[:, sub, :])], [("la", sub), "M3"], [kA3])
            P.actf(E[0][:], bA1[:], AF.Exp, [kA1], [("E", 0)])
            P.actf(E[1][:], bA1[:], AF.Exp, [kA1], [("E", 1)], scale=-1.0)
            P.actf(E[2][:], bA2[:], AF.Exp, [kA2], [("E", 2)])
            P.actf(E[3][:], bA3[:], AF.Exp, [kA3], [("E", 3)])
            e3 = lambda t: t[:].rearrange("p (h i) -> p h i", h=4)
            rq = [("qT", h) for h in range(4)]
            rk = [("kT", h) for h in range(4)]
            P.tt("dve", QpT[:, :, ts_], qT[:, :, ts_], e3(E[0]), ALU.mult, rq + [("E", 0)], [("QpT", sub)])
            P.tt("dve", KpT[:, :, ts_], kT[:, :, ts_], e3(E[1]), ALU.mult, rk + [("E", 1)], [("KpT", sub)])
            P.tt("dve", QppT[:, :, ts_], qT[:, :, ts_], e3(E[2]), ALU.mult, rq + [("E", 2)], [("QppT", sub)])
            P.tt("dve", Kpp[:, sub, :], ktok[:, sub, :], E[3][:], ALU.mult, [("ktok", sub), ("E", 3)], [("Kpp", sub)])
            P.copy("act", DEC[:, :, blk * 4 + sub], e3(E[2])[:, :, 127], [("E", 2)], ["DEC"])
            for hf in range(2):
                b, kb = bank()
                P.mm(b[:], [(C.hT[:, kt, ts_], w[:, kt, GR + hf * 512:GR + (hf + 1) * 512]) for kt in range(8)],
                     rh + kw, [kb])
                P.actf(sil[:], b[:], AF.Silu, [kb], ["sil"])
                P.stt(Rg[:, sub, hf * 512:(hf + 1) * 512], sil[:], 16.0, gob[:, hf * 512:(hf + 1) * 512],
                      ALU.mult, ALU.mult, ["sil", "gob"], [("Rg", sub)])
        fm = lambda ap: ap.rearrange("(h p) t -> p h t", p=128)[:, :, t0:t0 + 512]
        P.dma("pool", "stQ", [(fm(S["QpT"]), QpT[:]), (fm(S["KpT"]), KpT[:]), (fm(S["QppT"]), QppT[:])],
              reads=[(n, s_) for n in ("QpT", "KpT", "QppT") for s_ in range(4)])
        P.dma("pool", "stK", [(tok_view(S["Kpp"], t0), Kpp[:]), (tok_view(S["V"], t0), vsb[:]),
                              (tok_view(S["Rg"], t0), Rg[:])],
              reads=[(n, s_) for n in ("Kpp", "vsb", "Rg") for s_ in range(4)])
    P.dma("pool", "stD", [(S["DEC"], DEC[:].rearrange("p h c -> p (h c)"))], reads=["DEC"])
    P.end()


def emit_gla_scan(P, K, S, OT, T, with_out, Sbounce=None, Sgath=None, flag=None):
    P.begin()
    NCH = T // 128
    ngrp = NCH // 4
    St = P.sb("St", [128, 4, 256], F32)
    Sb = P.sb("Sb", [128, 4, 256], BF16)
    DEC = P.sb("DEC", [128, 4, NCH], F32)
    Kpp = [P.sb("Kpp%d" % i, [128, 4, 512], BF16) for i in range(2)]
    V = [P.sb("V%d" % i, [128, 4, 1024], BF16) for i in range(2)]
    pu = [P.ps("pu%d" % i, [128, 512], F32) for i in range(2)]
    P.dma("sp", "DEC", [(DEC[:].rearrange("p h c -> p (h c)"), S["DEC"])], writes=["DEC"])
    if with_out:
        QpT = [P.sb("QpT%d" % i, [128, 4, 512], BF16) for i in range(2)]
        KpT = [P.sb("KpT%d" % i, [128, 4, 512], BF16) for i in range(2)]
        QppT = [P.sb("QppT%d" % i, [128, 4, 512], BF16) for i in range(2)]
        Rg = [P.sb("Rg%d" % i, [128, 4, 1024], BF16) for i in range(2)]
        maskT = P.sb("maskT", [128, 128], F32)
        scm = [P.sb("scm%d" % i, [128, 128], BF16) for i in range(2)]
        og = [P.sb("og%d" % i, [128, 256], BF16) for i in range(2)]
        oT = [P.sb("oT%d" % i, [128, 8, 512], BF16) for i in range(2)]
        junk = P.sb("junk", [128, 256], BF16)
        ss = P.sb("ss", [128, 8], F32)
        sq = P.sb("sq", [128, 8], F32)
        rs = P.sb("rs", [128, 8], F32)
        mhalf = P.sb("mhalf", [128, 1], F32)
        flg = P.sb("flg", [128, 1], F32)
        ident = P.sb("ident", [128, 128], BF16)
        psc = [P.ps("psc%d" % i, [128, 512], F32) for i in range(2)]
        po = [P.ps("po%d" % i, [128, 512], F32) for i in range(2)]
        ptr = [P.ps("ptr%d" % i, [128, 1024], BF16) for i in range(2)]
        P.dma("sp", "maskT", [(maskT[:], K["TRI"])], writes=["maskT"])
        P.dma("pool", "ident", [(ident[:], K["ident"])], writes=["ident"])
        P.add("pool", lambda e: e.memset(mhalf[:], -0.5), [], ["mhalf"])
        P.dma("sp", "flg", [(flg[:], flag)], writes=["flg"])
        P.dma("sp", "St", [(St[:].rearrange("p h v -> p (h v)"), Sgath[0:128, :])], writes=["St"])
        P.ts("dve", St[:], St[:], flg[:, 0:1], None, ALU.mult, None, ["St", "flg"], [("S", h) for h in range(4)])
        for h in range(4):
            P.copy("act", Sb[:, h, :], St[:, h, :], [("S", h)], [("Sb", h)])
    else:
        P.add("dve", lambda e: e.memset(St[:], 0.0), [], [("S", h) for h in range(4)])
    fm = lambda ap, g: ap.rearrange("(h p) t -> p h t", p=128)[:, :, g * 512:(g + 1) * 512]
    OT_v = OT[0:1024, :].rearrange("(f p) t -> p f t", p=128) if with_out else None
    n = 0
    for g in range(ngrp):
        b2 = g % 2
        t0 = g * 512
        P.dma("sp", ("ldK", b2), [(Kpp[b2][:], tok_view(S["Kpp"], t0)), (V[b2][:], tok_view(S["V"], t0))],
              writes=[("Kpp", b2), ("V", b2)])
        if with_out:
            P.dma("sp", ("ldQ", b2), [(QpT[b2][:], fm(S["QpT"], g)), (KpT[b2][:], fm(S["KpT"], g)),
                                      (QppT[b2][:], fm(S["QppT"], g)), (Rg[b2][:], tok_view(S["Rg"], t0))],
                  writes=[("Q", b2)])
        for cc in range(4):
            c = g * 4 + cc
            ts_ = slice(cc * 128, (cc + 1) * 128)
            for hd in range(4):
                i2 = n % 2
                n += 1
                hs = slice(hd * 128, (hd + 1) * 128)
                vs = slice(hd * 256, (hd + 1) * 256)
                if with_out:
                    P.mm(psc[i2][:, 0:128], [(KpT[b2][:, hd, ts_], QpT[b2][:, hd, ts_])], [("Q", b2)], [("psc", i2)])
                    P.tt("dve", scm[i2][:], psc[i2][:, 0:128], maskT[:], ALU.mult, [("psc", i2), "maskT"], [("scm", i2)])
                    P.mm(po[i2][:, 0:256], [(QppT[b2][:, hd, ts_], Sb[:, hd, :]), (scm[i2][:], V[b2][:, cc, vs])],
                         [("Q", b2), ("Sb", hd), ("scm", i2), ("V", b2)], [("po", i2)])
                P.mm(pu[i2][:, 0:256], [(Kpp[b2][:, cc, hs], V[b2][:, cc, vs])], [("Kpp", b2), ("V", b2)], [("pu", i2)])
                P.stt(St[:, hd, :], St[:, hd, :], DEC[:, hd, c:c + 1], pu[i2][:, 0:256], ALU.mult, ALU.add,
                      [("S", hd), "DEC", ("pu", i2)], [("S", hd)])
                if with_out:
                    P.copy("act", Sb[:, hd, :], St[:, hd, :], [("S", hd)], [("Sb", hd)])
                    k8 = n % 8
                    P.add("act", lambda e, o=junk[:], i=po[i2][:, 0:256], a=ss[:, k8:k8 + 1]:
                          e.activation(out=o, in_=i, func=AF.Square, accum_out=a), [("po", i2)], ["junk", ("ss", k8)])
                    P.ts("pool", sq[:, k8:k8 + 1], ss[:, k8:k8 + 1], 256.0 * EPS, None, ALU.add, None,
                         [("ss", k8)], [("sq", k8)])
                    P.tt("pool", rs[:, k8:k8 + 1], sq[:, k8:k8 + 1], mhalf[:, 0:1], ALU.pow,
                         [("sq", k8), "mhalf"], [("rs", k8)])
                    P.stt(og[i2][:], po[i2][:, 0:256], rs[:, k8:k8 + 1], Rg[b2][:, cc, vs], ALU.mult, ALU.mult,
                          [("po", i2), ("rs", k8), ("Q", b2)], [("og", i2)])
                    for vh in range(2):
                        P.tr(ptr[i2][:, vh * 128:(vh + 1) * 128], og[i2][:, vh * 128:(vh + 1) * 128], ident[:],
                             [("og", i2), "ident"], [("ptr", i2)])
                    P.copy("pool" if False else "dve", oT[b2][:, hd * 2:hd * 2 + 2, ts_],
                           ptr[i2][:, 0:256].rearrange("p (v i) -> p v i", v=2), [("ptr", i2)], [("oT", b2)])
        if with_out:
            P.dma("pool", ("stO", b2), [(OT_v[:, :, t0:t0 + 512], oT[b2][:])], reads=[("oT", b2)])
    if not with_out:
        P.dma("pool", "stS", [(Sbounce, St[:].rearrange("p h v -> p (h v)"))], reads=[("S", h) for h in range(4)])
    P.end()


def emit_mla_proj(P, K, X, win, gmix, gq384, gkv256, wuq, wukv, gqn, gqr2, gkn, gkr2, cosT, sinT, S, T):
    P.begin()
    C = Ctx()
    nblk = T // 512
    alloc_xin(P, C, K, gmix)
    alloc_fnorm(P, C)
    w = P.sb("w", [128, 8, 768], BF16)
    uq = P.sb("uq", [128, 3, 2048], BF16)
    ukn = P.sb("ukn", [128, 2, 1024], BF16)
    ukv = P.sb("ukv", [128, 2, 1024], BF16)
    g3 = P.sb("g3", [128, 3], F32)
    g2 = P.sb("g2", [128, 2], F32)
    gqn_s = P.sb("gqn", [128, 1], F32)
    gkn_s = P.sb("gkn", [128, 1], F32)
    gqr_s = P.sb("gqr", [64, 2], F32)
    gkr_s = P.sb("gkr", [64, 2], F32)
    cs = [P.sb("cs%d" % i, [64, 512], F32) for i in range(2)]
    sn = [P.sb("sn%d" % i, [64, 512], F32) for i in range(2)]
    cqn = P.sb("cqn", [128, 3, 512], BF16)
    ckn = P.sb("ckn", [128, 2, 512], BF16)
    ra = P.sb("ra", [64, 512], F32)
    rb = P.sb("rb", [64, 512], F32)
    QT = [P.sb("QT%d" % i, [128, 8, 512], BF16) for i in range(2)]
    QR = [P.sb("QR%d" % i, [64, 8, 512], BF16) for i in range(2)]
    KT = [P.sb("KT%d" % i, [128, 8, 512], BF16) for i in range(2)]
    KR = [P.sb("KR%d" % i, [64, 512], BF16) for i in range(2)]
    Vs = [P.sb("Vs%d" % i, [128, 4, 1024], BF16) for i in range(2)]
    bk = [P.ps("bk%d" % i, [128, 512], F32) for i in range(5)]
    win_v = win.rearrange("(kt p) f -> p kt f", p=128)
    kw = load_w(P, "w", w[:, :, 0:704], win_v, 0, 704)
    P.dma("pool", ("w", 9), [(w[:, kt, 704:736], win_v[:, kt, 672:704]) for kt in range(8)]
          + [(w[:, kt, 736:768], win_v[:, kt, 640:672]) for kt in range(8)], writes=[("w", 9)])
    kw.append(("w", 9))
    uq_v = wuq.rearrange("(ct p) (h f) -> p ct h f", p=128, h=8)
    uq_d = uq[:].rearrange("p ct (h f) -> p ct h f", h=8)
    P.dma("pool", "uq", [(uq_d[:, ct, :, 0:192], uq_v[:, ct, :, :]) for ct in range(3)]
          + [(uq_d[:, ct, :, 192:224], uq_v[:, ct, :, 160:192]) for ct in range(3)]
          + [(uq_d[:, ct, :, 224:256], uq_v[:, ct, :, 128:160]) for ct in range(3)], writes=["uq"])
    ukv_v = wukv.rearrange("(ct p) (h f) -> p ct h f", p=128, h=8)
    P.dma("pool", "ukv", [(ukn[:, ct, :].rearrange("p (h f) -> p h f", h=8), ukv_v[:, ct, :, 0:128]) for ct in range(2)]
          + [(ukv[:, ct, :].rearrange("p (h f) -> p h f", h=8), ukv_v[:, ct, :, 128:256]) for ct in range(2)],
          writes=["ukv"])
    for (t, src, k) in ((g3, gq384, "g3"), (g2, gkv256, "g2"), (gqn_s, gqn, "gqn"), (gkn_s, gkn, "gkn"),
                        (gqr_s, gqr2, "gqr"), (gkr_s, gkr2, "gkr")):
        P.dma("sp", k, [(t[:], src)], writes=[k])
    rh = [("hT", kt) for kt in range(8)]
    nb = [0]

    def bank():
        i = nb[0] % 5
        nb[0] += 1
        return bk[i], ("bk", i)

    def rope(src_a, ka, src_b, kb_, g2col, out, kout, b2, gkey):
        fnorm(P, C, [(src_a, ka)], 64, 64.0, 512,
              [(ra[:], g2col[:, 0:1], src_a, ka, "ra"), (rb[:], g2col[:, 1:2], src_b, kb_, "rb")], [gkey])
        P.tt("dve", ra[:], ra[:], cs[b2][:], ALU.mult, ["ra", ("cs", b2)], ["ra"])
        P.tt("dve", rb[:], rb[:], sn[b2][:], ALU.mult, ["rb", ("cs", b2)], ["rb"])
        P.tt("dve", out, ra[:], rb[:], ALU.add, ["ra", "rb"], [kout])

    load_norm(P, C, X, 0, "x")
    for blk in range(nblk):
        t0 = blk * 512
        b2 = blk % 2
        transpose_hT(P, C, C.gvec)
        if blk + 1 < nblk:
            load_norm(P, C, X, blk + 1, "x")
        P.dma("sp", ("cs", b2), [(cs[b2][:], cosT[:, t0:t0 + 512]), (sn[b2][:], sinT[:, t0:t0 + 512])],
              writes=[("cs", b2)])
        srcs = []
        for ct in range(3):
            b, kb = bank()
            P.mm(b[:], [(w[:, kt, ct * 128:(ct + 1) * 128], C.hT[:, kt, :]) for kt in range(8)], rh + kw, [kb])
            srcs.append((b[:], kb))
        fnorm(P, C, srcs, 128, 384.0, 512,
              [(cqn[:, ct, :], g3[:, ct:ct + 1], srcs[ct][0], srcs[ct][1], ("cqn", ct)) for ct in range(3)], ["g3"])
        srcs = []
        for ct in range(2):
            b, kb = bank()
            P.mm(b[:], [(w[:, kt, 384 + ct * 128:384 + (ct + 1) * 128], C.hT[:, kt, :]) for kt in range(8)], rh + kw, [kb])
            srcs.append((b[:], kb))
        fnorm(P, C, srcs, 128, 256.0, 512,
              [(ckn[:, ct, :], g2[:, ct:ct + 1], srcs[ct][0], srcs[ct][1], ("ckn", ct)) for ct in range(2)], ["g2"])
        ba, ka = bank()
        P.mm(ba[0:64, :], [(w[:, kt, 640:704], C.hT[:, kt, :]) for kt in range(8)], rh + kw, [ka])
        bb, kbb = bank()
        P.mm(bb[0:64, :], [(w[:, kt, 704:768], C.hT[:, kt, :]) for kt in range(8)], rh + kw, [kbb])
        rope(ba[0:64, :], ka, bb[0:64, :], kbb, gkr_s, KR[b2][:], ("KR", b2), b2, "gkr")
        rcq = [("cqn", ct) for ct in range(3)] + ["uq"]
        rck = [("ckn", ct) for ct in range(2)] + ["ukv"]
        for hd in range(8):
            o = hd * 256
            b, kb = bank()
            P.mm(b[:], [(uq[:, ct, o:o + 128], cqn[:, ct, :]) for ct in range(3)], rcq, [kb])
            fnorm(P, C, [(b[:], kb)], 128, 128.0, 512, [(QT[b2][:, hd, :], gqn_s[:, 0:1], b[:], kb, ("QT", b2))], ["gqn"])
            ba, ka = bank()
            P.mm(ba[0:64, :], [(uq[:, ct, o + 128:o + 192], cqn[:, ct, :]) for ct in range(3)], rcq, [ka])
            bb, kbb = bank()
            P.mm(bb[0:64, :], [(uq[:, ct, o + 192:o + 256], cqn[:, ct, :]) for ct in range(3)], rcq, [kbb])
            rope(ba[0:64, :], ka, bb[0:64, :], kbb, gqr_s, QR[b2][:, hd, :], ("QR", b2), b2, "gqr")
            b, kb = bank()
            P.mm(b[:], [(ukn[:, ct, hd * 128:(hd + 1) * 128], ckn[:, ct, :]) for ct in range(2)], rck, [kb])
            fnorm(P, C, [(b[:], kb)], 128, 128.0, 512, [(KT[b2][:, hd, :], gkn_s[:, 0:1], b[:], kb, ("KT", b2))], ["gkn"])
        for sub in range(4):
            for hf in range(2):
                b, kb = bank()
                P.mm(b[:], [(ckn[:, ct, sub * 128:(sub + 1) * 128], ukv[:, ct, hf * 512:(hf + 1) * 512])
                            for ct in range(2)], rck, [kb])
                P.copy("act", Vs[b2][:, sub, hf * 512:(hf + 1) * 512], b[:], [kb], [("Vs", b2)])
        QT_v = S["QT"].rearrange("(h f) t -> f h t", f=192)
        P.dma("pool", ("stq", b2), [(QT_v[0:128, :, t0:t0 + 512], QT[b2][:]), (QT_v[128:192, :, t0:t0 + 512], QR[b2][:])],
              reads=[("QT", b2), ("QR", b2)])
        P.dma("pool", ("stk", b2), [(S["KTh"][h][:, t0:t0 + 512], KT[b2][:, h, :]) for h in range(8)]
              + [(S["KR"][:, t0:t0 + 512], KR[b2][:]), (tok_view(S["Vb"][blk], 0), Vs[b2][:])],
              reads=[("KT", b2), ("KR", b2), ("Vs", b2)])
    P.end()


def emit_mla_attn(P, K, S, G, OT, negb, T):
    P.begin()
    nqb = T // 512
    nkp = T // 128
    Kn = [P.sb("Kn%d" % i, [128, 2, T], BF16) for i in range(2)]
    Kr = P.sb("Kr", [128, 2, T], BF16)
    V = [P.sb("V%d" % i, [128, 2 * nkp, 130], BF16) for i in range(2)]
    Qn = [P.sb("Qn%d" % i, [128, T], BF16) for i in range(2)]
    Qr = [P.sb("Qr%d" % i, [128, T], BF16) for i in range(2)]
    msk = P.sb("msk", [128, 4, 512], BF16)
    ident = P.sb("ident", [128, 128], BF16)
    nb = P.sb("nb", [128, 1], F32)
    PT = [P.sb("PT%d" % i, [128, 512], BF16) for i in range(3)]
    rl = P.sb("rl", [128, 4], F32)
    on = P.sb("on", [128, 4, 128], BF16)
    oT = [P.sb("oT%d" % i, [128, 512], BF16) for i in range(2)]
    psT = [P.ps("psT%d" % i, [128, 512], F32) for i in range(3)]
    po = [P.ps("po%d" % i, [128, 512], F32) for i in range(4)]
    ptr = P.ps("ptr", [128, 1024], BF16)
    P.dma("pool", "msk", [(msk[:], K["cmask"])], writes=["msk"])
    P.dma("pool", "ident", [(ident[:], K["ident"])], writes=["ident"])
    P.add("pool", lambda e: e.memset(Kr[64:128, :, :], 0.0), [], ["Krz"])
    for i in range(2):
        P.add("pool", lambda e, i=i: e.memset(Qr[i][64:128, :], 0.0), [], [("Qrz", i)])
        P.add("pool", lambda e, i=i: e.memset(V[i][:, :, 128:130], 1.0), [], [("Vone", i)])
    P.dma("sp", "nb", [(nb[:], negb)], writes=["nb"])
    P.dma("sp", "Kr", [(Kr[0:64, 0, :], G["KR"][0:64, :]), (Kr[0:64, 1, :], S["KR"])], writes=["Kr"])
    QT_v = S["QT"].rearrange("(h f) t -> f h t", f=192)
    scale = 192.0 ** -0.5
    LOOK = 2
    units = []
    u = 0
    for hd in range(8):
        for qb in range(nqb):
            kbs = [(0, kb) for kb in range(nkp)] + [(1, kb) for kb in range(4 * (qb + 1))]
            for idx, (half, kb) in enumerate(kbs):
                units.append((hd, qb, half, kb, idx == 0, idx == len(kbs) - 1, u % 2))
            u += 1
    loaded = set()

    def load_head(hd):
        if hd in loaded or hd >= 8:
            return
        loaded.add(hd)
        h2 = hd % 2
        cs_ = slice(hd * 128, (hd + 1) * 128)
        P.dma("sp", ("ldk", h2), [(Kn[h2][:, 0, :], G["KTh"][hd][0:128, :]),
                                  (Kn[h2][:, 1, :], S["KTh"][hd]),
                                  (Qn[h2][:], QT_v[0:128, hd, :]), (Qr[h2][0:64, :], QT_v[128:192, hd, :])],
              writes=[("K", h2)])
        vpairs = []
        for vb in range(nqb):
            k0 = vb * 4
            vpairs.append((V[h2][:, k0:k0 + 4, 0:128], G["Vb"][vb][0:512, cs_].rearrange("(k p) c -> p k c", p=128)))
            vpairs.append((V[h2][:, nkp + k0:nkp + k0 + 4, 0:128],
                           S["Vb"][vb][:, cs_].rearrange("(k p) c -> p k c", p=128)))
        P.dma("sp", ("ldv", h2), vpairs, writes=[("V", h2)])

    def score(n):
        hd, qb, half, kb, first, last, j = units[n]
        h2 = hd % 2
        i3 = n % 3
        qs = slice(qb * 512, (qb + 1) * 512)
        ks = slice(kb * 128, (kb + 1) * 128)
        load_head(hd)
        P.mm(psT[i3][:], [(Kn[h2][:, half, ks], Qn[h2][:, qs]), (Kr[:, half, ks], Qr[h2][:, qs])],
             [("K", h2), "Kr", "Krz", ("Qrz", h2)], [("psT", i3)])

    load_head(0)
    for n in range(min(LOOK, len(units))):
        score(n)
    for n in range(len(units)):
        hd, qb, half, kb, first, last, j = units[n]
        h2 = hd % 2
        i3 = n % 3
        qs = slice(qb * 512, (qb + 1) * 512)
        cs_ = slice(hd * 128, (hd + 1) * 128)
        if half == 0:
            P.actf(PT[i3][:], psT[i3][:], AF.Exp, [("psT", i3), "nb"], [("PT", i3)], scale=scale, bias=nb[:, 0:1])
        else:
            P.actf(PT[i3][:], psT[i3][:], AF.Exp, [("psT", i3)], [("PT", i3)], scale=scale)
            if kb >= 4 * qb:
                P.tt("pool", PT[i3][:], PT[i3][:], msk[:, kb - 4 * qb, :], ALU.mult, [("PT", i3), "msk"], [("PT", i3)])
        if n + LOOK < len(units):
            score(n + LOOK)
        for sub in range(4):
            P.mm1(po[sub][:, 0:129], PT[i3][:, sub * 128:(sub + 1) * 128], V[h2][:, half * nkp + kb, 0:129],
                  first, last, [("PT", i3), ("V", h2), ("Vone", h2)], [("po", sub)])
        if last:
            for sub in range(4):
                P.add("dve", lambda e, o=rl[:, sub:sub + 1], i=po[sub][:, 128:129]: e.reciprocal(out=o, in_=i),
                      [("po", sub)], [("rl", sub)])
                P.ts("dve", on[:, sub, :], po[sub][:, 0:128], rl[:, sub:sub + 1], None, ALU.mult, None,
                     [("po", sub), ("rl", sub)], [("on", sub)])
                P.tr(ptr[:, sub * 128:(sub + 1) * 128], on[:, sub, :], ident[:], [("on", sub), "ident"], ["ptr"])
            P.copy("act", oT[j][:], ptr[:, 0:512], ["ptr"], [("oT", j)])
            P.dma("pool", ("sto", j), [(OT[cs_, qs], oT[j][:])], reads=[("oT", j)])
            if qb == 1:
                load_head(hd + 1)
    P.end()


BIG = ["ffn1_w_gate", "ffn1_w_up", "ffn1_w_down", "ffn2_w_gate", "ffn2_w_up", "ffn2_w_down", "w_out", "mem_w_kv",
       "gla_w_in", "gla_w_alpha", "mla_w_in", "mla_w_uq", "mla_w_ukv"]


def host_layout(inp, T, n_cores):
    f = lambda a: np.ascontiguousarray(np.asarray(a, dtype=np.float32))
    sh = {}
    for k in BIG:
        sh[k] = f(inp[k])
    pk = lambda a: f(np.asarray(a).reshape(a.shape[0], -1, 128).transpose(0, 2, 1))
    for k in ("ffn1_norm", "ffn2_norm", "mix_norm", "mem_norm", "mla_q_norm", "mla_kv_norm",
              "memq_norm", "memk_norm", "mla_qn_norm", "mla_kn_norm"):
        sh[k] = pk(np.asarray(inp[k]))
    for k in ("mla_qr_norm", "mla_kr_norm"):
        g = np.asarray(inp[k], dtype=np.float32)
        sh[k] = f(np.stack([g, np.concatenate([g[:, 32:], g[:, :32]], axis=1)], axis=2))
    ba = np.asarray(inp["gla_b_alpha"], dtype=np.float32)
    sh["gla_b_alpha"] = f(np.broadcast_to(ba[:, None, :], (ba.shape[0], 128, 512)))
    go = np.tile(np.asarray(inp["gla_out_norm"], dtype=np.float32), (1, 4))
    sh["gla_out_norm"] = f(np.broadcast_to(go[:, None, :], (go.shape[0], 128, 1024)))
    j = np.arange(128)
    tri = (j[:, None] <= j[None, :]).astype(np.float32)
    sh["c_ident"] = np.eye(128, dtype=np.float32)
    sh["c_TRI"] = tri
    sh["c_R1"] = f(tri - (j[:, None] <= 63).astype(np.float32))
    sh["c_M3"] = (j[:, None] > j[None, :]).astype(np.float32)
    qq = np.arange(512)
    sh["c_cmask"] = f(np.stack([(jd * 128 + j[:, None] <= qq[None, :]) for jd in range(4)], axis=1).astype(np.float32))
    x = np.asarray(inp["x"], dtype=np.float32)
    mem = np.asarray(inp["mem"], dtype=np.float32)
    inv = (1.0 / (np.float32(10000.0) ** (np.arange(0, 64, 2, dtype=np.float32) / np.float32(64)))).astype(np.float32)
    maps = []
    for c in range(n_cores):
        b, hf = c // 2, c % 2
        m = dict(sh)
        m["x"] = f(x[b, hf * T:(hf + 1) * T])
        m["mem"] = f(mem[b])
        pos = np.arange(hf * T, (hf + 1) * T, dtype=np.float32)
        ang = (pos[None, :] * inv[:, None]).astype(np.float32)
        cs, sn = np.cos(ang).astype(np.float32), np.sin(ang).astype(np.float32)
        m["c_cos"] = f(np.concatenate([cs, cs], axis=0))
        m["c_sin"] = f(np.concatenate([-sn, sn], axis=0))
        m["c_flag"] = np.full((128, 1), float(hf), np.float32)
        m["c_negb"] = np.full((128, 1), 0.0 if hf else -30000.0, np.float32)
        maps.append(m)
    return maps


def build_program(T, depth, maps0, n_cores):
    nc = bass.Bass("TRN2", target_bir_lowering=False)
    I = {}
    for k, v in maps0.items():
        I[k] = nc.dram_tensor(k, list(v.shape), F32, kind="ExternalInput").ap()
    out = nc.dram_tensor("out", [T, D], F32, kind="ExternalOutput").ap()
    NCH = T // 128

    def scr(name, shape, dt):
        return nc.dram_tensor(name, shape, dt)
    X = scr("X", [T, D], F32).ap()
    OT = scr("OT", [1536, T], BF16).ap()
    SG = {k: scr("g_" + k, [512, T], BF16).ap() for k in ("QpT", "KpT", "QppT")}
    SG["Kpp"] = scr("g_Kpp", [T, 512], BF16).ap()
    SG["V"] = scr("g_V", [T, 1024], BF16).ap()
    SG["Rg"] = scr("g_Rg", [T, 1024], BF16).ap()
    SG["DEC"] = scr("g_DEC", [128, 4 * NCH], F32).ap()
    Sbn = scr("g_Sb", [128, 1024], F32)
    Sgt = scr("g_Sg", [256, 1024], F32)
    nblk = T // 512
    SMt = {"KTh": [scr("m_KT%d" % h, [128, T], BF16) for h in range(8)], "KR": scr("m_KR", [64, T], BF16),
           "Vb": [scr("m_V%d" % b, [512, 1024], BF16) for b in range(nblk)]}
    GMt = {"KTh": [scr("m_KTg%d" % h, [256, T], BF16) for h in range(8)], "KR": scr("m_KRg", [128, T], BF16),
           "Vb": [scr("m_Vg%d" % b, [1024, 1024], BF16) for b in range(nblk)]}
    SM = {"QT": scr("m_QT", [1536, T], BF16).ap(), "KTh": [t.ap() for t in SMt["KTh"]], "KR": SMt["KR"].ap(),
          "Vb": [t.ap() for t in SMt["Vb"]]}
    GM = {"KTh": [t.ap() for t in GMt["KTh"]], "KR": GMt["KR"].ap(), "Vb": [t.ap() for t in GMt["Vb"]]}
    groups = [[2 * i, 2 * i + 1] for i in range(n_cores // 2)]
    K = {"ident": I["c_ident"], "TRI": I["c_TRI"], "R1": I["c_R1"], "M3": I["c_M3"], "cmask": I["c_cmask"]}
    with ExitStack() as es:
        P = Prog(nc, es)
        for i in range(depth):
            j = i // 2
            src = I["x"] if i == 0 else X
            emit_ffn(P, K, src, X, I["ffn1_w_gate"][i], I["ffn1_w_up"][i], I["ffn1_w_down"][i], I["ffn1_norm"][i], T)
            if i % 2 == 0:
                emit_gla_proj(P, K, X, I["gla_w_in"][j], I["mix_norm"][i], I["gla_w_alpha"][j], I["gla_b_alpha"][j],
                              I["gla_out_norm"][j], SG, T)
                win_v = I["gla_w_in"][j].rearrange("(kt p) f -> p kt f", p=128)
                emit_gla_scan(P, K, SG, OT, T, False, Sbounce=Sbn.ap())
                emit_mem(P, K, X, OT, win_v, 3088, I["mix_norm"][i], I["mem"], I["mem_norm"][i], I["mem_w_kv"][i],
                         I["memq_norm"][i], I["memk_norm"][i], T,
                         pre=lambda: _cc(P, "ccS", Sbn.ap().opt(), Sgt.ap().opt(), groups, [], []))
                emit_gla_scan(P, K, SG, OT, T, True, Sgath=Sgt.ap(), flag=I["c_flag"])
            else:
                emit_mla_proj(P, K, X, I["mla_w_in"][j], I["mix_norm"][i], I["mla_q_norm"][j], I["mla_kv_norm"][j],
                              I["mla_w_uq"][j], I["mla_w_ukv"][j], I["mla_qn_norm"][j], I["mla_qr_norm"][j],
                              I["mla_kn_norm"][j], I["mla_kr_norm"][j], I["c_cos"], I["c_sin"], SM, T)
                win_v = I["mla_w_in"][j].rearrange("(kt p) f -> p kt f", p=128)
                pieces = [(SMt["KR"], GMt["KR"])] + list(zip(SMt["KTh"], GMt["KTh"])) + list(zip(SMt["Vb"], GMt["Vb"]))

                def pre_cc(pieces=pieces):
                    for ci, (a, g) in enumerate(pieces):
                        _cc(P, ("ccM", ci), a.ap().opt(), g.ap().opt(), groups, [], [])
                emit_mem(P, K, X, OT, win_v, 704, I["mix_norm"][i], I["mem"], I["mem_norm"][i], I["mem_w_kv"][i],
                         I["memq_norm"][i], I["memk_norm"][i], T, pre=pre_cc)
                emit_mla_attn(P, K, SM, GM, OT, I["c_negb"], T)
            emit_outproj(P, K, X, OT, I["w_out"][i], T)
            dst = out if i == depth - 1 else X
            emit_ffn(P, K, X, dst, I["ffn2_w_gate"][i], I["ffn2_w_up"][i], I["ffn2_w_down"][i], I["ffn2_norm"][i], T)
        nops = P.n_ops
    return nc, nops


def kernel(**inputs):
    n_cores = 8
    B, S, _ = inputs["x"].shape
    T = S * B // n_cores
    maps = host_layout(inputs, T, n_cores)
    nc, _ = build_program(T, 4, maps[0], n_cores)
    res = run_bass_kernel_spmd(nc, maps, core_ids=list(range(n_cores)))
    y = np.empty((B, S, D), np.float32)
    for c in range(n_cores):
        y[c // 2, (c % 2) * T:(c % 2 + 1) * T] = res.results[c]["out"]
    return y
```
